# Optimizing a Trainium2 kernel written in Bass

```python
import math
import jax, jax.numpy as jnp
from jax import lax
import numpy as np

D_MODEL = 1024
BATCH = 16
SEQ = 4096
DEPTH = 4

F32 = jnp.float32
N_BRANCHES = 4
BRANCH_WIDTH = 512
NORM_EPS = 1e-6
CHUNK = 64
GLA_HEADS = 4
GLA_DK = 64
GLA_DV = BRANCH_WIDTH // GLA_HEADS
GLA_KEY = GLA_HEADS * GLA_DK
GLA_RANK = 16
GLA_GATE_TEMP = 16.0
RET_HEADS = 4
RET_DK = 64
RET_DV = BRANCH_WIDTH // RET_HEADS
RET_KEY = RET_HEADS * RET_DK
ROPE_BASE = 10000.0
LRU_BLOCKS = 8
LRU_BLOCK_DIM = BRANCH_WIDTH // LRU_BLOCKS
CONV_WIDTH = 4
LRU_C = 8.0
S5_GROUP = 16
S5_GROUPS = BRANCH_WIDTH // S5_GROUP
S5_STATE = 64
S5_BLOCK = 128
IN_SIZES = (GLA_KEY, GLA_KEY, BRANCH_WIDTH, GLA_RANK, BRANCH_WIDTH,
            RET_KEY, RET_KEY, BRANCH_WIDTH, BRANCH_WIDTH,
            BRANCH_WIDTH, BRANCH_WIDTH,
            BRANCH_WIDTH, BRANCH_WIDTH)
IN_COLS = sum(IN_SIZES)

kernel_name = 'hybrid_gla_retnet_rglru_s5_gated_merge'


def rmsnorm(x, gain):
    xf = x.astype(F32)
    y = xf * lax.rsqrt(jnp.mean(xf * xf, axis=-1, keepdims=True) + NORM_EPS)
    return (y * gain.astype(F32)).astype(x.dtype)


def split_columns(proj):
    offsets, acc = [], 0
    for s in IN_SIZES[:-1]:
        acc += s
        offsets.append(acc)
    return jnp.split(proj, offsets, axis=-1)


def to_chunks(t, heads):
    b, s, w = t.shape
    return t.reshape(b, s // CHUNK, CHUNK, heads, w // heads).transpose(0, 3, 1, 2, 4)


def from_chunks(t):
    b, h, n, c, d = t.shape
    return t.transpose(0, 2, 3, 1, 4).reshape(b, n * c, h * d)


def scan_chunk_states(kv, decay):
    def step(state, inp):
        kv_n, dec_n = inp
        return state * dec_n + kv_n, state
    init = jnp.zeros(kv.shape[:2] + kv.shape[3:], F32)
    _, prev = lax.scan(step, init, (jnp.moveaxis(kv, 2, 0), jnp.moveaxis(decay, 2, 0)))
    return jnp.moveaxis(prev, 0, 2)


def gla_mix(q, k, v, lr, w_lr, b_lr, norm_gain):
    z = jnp.einsum('bsr,rk->bsk', lr.astype(F32), w_lr.astype(F32)) + b_lr.astype(F32)
    log_a = jax.nn.log_sigmoid(z) / GLA_GATE_TEMP
    qc = to_chunks(q.astype(F32), GLA_HEADS) * GLA_DK ** -0.5
    kc = to_chunks(k.astype(F32), GLA_HEADS)
    vc = to_chunks(v.astype(F32), GLA_HEADS)
    cum = jnp.cumsum(to_chunks(log_a, GLA_HEADS), axis=3)
    last = cum[..., -1:, :]
    q_dec = qc * jnp.exp(cum)
    k_dec = kc * jnp.exp(-cum)
    k_tail = kc * jnp.exp(last - cum)
    causal = jnp.tril(jnp.ones((CHUNK, CHUNK), dtype=bool))
    scores = jnp.where(causal, jnp.einsum('bhnid,bhnjd->bhnij', q_dec, k_dec), 0.0)
    o = jnp.einsum('bhnij,bhnjv->bhniv', scores, vc)
    kv = jnp.einsum('bhncd,bhncv->bhndv', k_tail, vc)
    s_prev = scan_chunk_states(kv, jnp.exp(last[..., 0, :])[..., None])
    o = o + jnp.einsum('bhncd,bhndv->bhncv', q_dec, s_prev)
    o = o * lax.rsqrt(jnp.mean(o * o, axis=-1, keepdims=True) + NORM_EPS)
    o = o * norm_gain.astype(F32).reshape(GLA_HEADS, 1, 1, GLA_DV)
    return from_chunks(o)


def rotary(t):
    s, d = t.shape[1], t.shape[-1]
    half = d // 2
    inv = ROPE_BASE ** (-jnp.arange(half, dtype=F32) / half)
    ang = jnp.arange(s, dtype=F32)[:, None] * inv[None, :]
    cos = jnp.cos(ang)[None, :, None, :]
    sin = jnp.sin(ang)[None, :, None, :]
    t1, t2 = t[..., :half], t[..., half:]
    return jnp.concatenate([t1 * cos - t2 * sin, t1 * sin + t2 * cos], axis=-1)


def retention_mix(q, k, v, gn_gain, gn_bias):
    b, s, _ = q.shape
    n = s // CHUNK
    qr = rotary(q.astype(F32).reshape(b, s, RET_HEADS, RET_DK)).reshape(b, s, RET_KEY)
    kr = rotary(k.astype(F32).reshape(b, s, RET_HEADS, RET_DK)).reshape(b, s, RET_KEY) * RET_DK ** -0.5
    qc = to_chunks(qr, RET_HEADS)
    kc = to_chunks(kr, RET_HEADS)
    vc = to_chunks(v.astype(F32), RET_HEADS)
    log_gamma = jnp.log1p(-jnp.exp2(-5.0 - jnp.arange(RET_HEADS, dtype=F32)))
    pos = jnp.arange(CHUNK, dtype=F32)
    rel = pos[:, None] - pos[None, :]
    intra = jnp.where(rel >= 0, jnp.exp(jnp.maximum(rel, 0.0)[None] * log_gamma[:, None, None]), 0.0)
    scores = jnp.einsum('bhnid,bhnjd->bhnij', qc, kc) * intra[:, None]
    o = jnp.einsum('bhnij,bhnjv->bhniv', scores, vc)
    k_w = jnp.exp((CHUNK - 1.0 - pos)[None, :] * log_gamma[:, None])
    kv = jnp.einsum('bhncd,hc,bhncv->bhndv', kc, k_w, vc)
    chunk_decay = jnp.broadcast_to(jnp.exp(CHUNK * log_gamma)[None, :, None, None, None], (1, RET_HEADS, n, 1, 1))
    s_prev = scan_chunk_states(kv, chunk_decay)
    q_w = jnp.exp((pos + 1.0)[None, :] * log_gamma[:, None])
    o = o + jnp.einsum('bhncd,bhndv,hc->bhncv', qc, s_prev, q_w)
    mean = jnp.mean(o, axis=-1, keepdims=True)
    var = jnp.mean(jnp.square(o - mean), axis=-1, keepdims=True)
    o = (o - mean) * lax.rsqrt(var + NORM_EPS)
    o = o * gn_gain.astype(F32).reshape(RET_HEADS, 1, 1, RET_DV) + gn_bias.astype(F32).reshape(RET_HEADS, 1, 1, RET_DV)
    return from_chunks(o)


def linear_combine(left, right):
    a1, b1 = left
    a2, b2 = right
    return a1 * a2, a2 * b1 + b2


def rglru_mix(x, conv_w, conv_b, w_a, b_a, w_x, b_x, lam):
    b, s, w = x.shape
    xf = x.astype(F32)
    xc = lax.conv_general_dilated(xf, conv_w.astype(F32)[:, None, :], window_strides=(1,),
                                  padding=[(CONV_WIDTH - 1, 0)], dimension_numbers=('NWC', 'WIO', 'NWC'),
                                  feature_group_count=w) + conv_b.astype(F32)
    xb = xc.reshape(b, s, LRU_BLOCKS, LRU_BLOCK_DIM)
    r = jax.nn.sigmoid(jnp.einsum('bsni,nij->bsnj', xb, w_a.astype(F32)).reshape(b, s, w) + b_a.astype(F32))
    i = jax.nn.sigmoid(jnp.einsum('bsni,nij->bsnj', xb, w_x.astype(F32)).reshape(b, s, w) + b_x.astype(F32))
    log_a = -LRU_C * r * jax.nn.softplus(-lam.astype(F32))
    a = jnp.exp(log_a)
    gated = xc * i * jnp.sqrt(-jnp.expm1(2.0 * log_a))
    _, h = lax.associative_scan(linear_combine, (a, gated), axis=1)
    return h


def complex_linear_combine(left, right):
    ar1, ai1, br1, bi1 = left
    ar2, ai2, br2, bi2 = right
    return (ar1 * ar2 - ai1 * ai2, ar1 * ai2 + ai1 * ar2,
            ar2 * br1 - ai2 * bi1 + br2, ar2 * bi1 + ai2 * br1 + bi2)


def s5_scan(u, a_re, a_im, b_re, b_im, c_re, c_im, d, log_dt):
    b, s, w = u.shape
    a_re, a_im = a_re.astype(F32), a_im.astype(F32)
    b_re, b_im = b_re.astype(F32), b_im.astype(F32)
    c_re, c_im = c_re.astype(F32), c_im.astype(F32)
    step = jnp.exp(log_dt.astype(F32))[:, None]
    mag = jnp.exp(step * a_re)
    ab_re = mag * jnp.cos(step * a_im)
    ab_im = mag * jnp.sin(step * a_im)
    den = a_re * a_re + a_im * a_im
    f_re = ((ab_re - 1.0) * a_re + ab_im * a_im) / den
    f_im = (ab_im * a_re - (ab_re - 1.0) * a_im) / den
    bb_re = f_re[..., None] * b_re - f_im[..., None] * b_im
    bb_im = f_re[..., None] * b_im + f_im[..., None] * b_re
    d_g = d.astype(F32).reshape(S5_GROUPS, S5_GROUP)
    a_blk_re = jnp.broadcast_to(ab_re, (b, S5_BLOCK, S5_GROUPS, S5_STATE))
    a_blk_im = jnp.broadcast_to(ab_im, (b, S5_BLOCK, S5_GROUPS, S5_STATE))
    ub = jnp.moveaxis(u.astype(F32).reshape(b, s // S5_BLOCK, S5_BLOCK, S5_GROUPS, S5_GROUP), 1, 0)

    def block_step(carry, u_blk):
        h_re, h_im = carry
        bu_re = jnp.einsum('blgi,gpi->blgp', u_blk, bb_re)
        bu_im = jnp.einsum('blgi,gpi->blgp', u_blk, bb_im)
        bu_re = bu_re.at[:, 0].add(ab_re * h_re - ab_im * h_im)
        bu_im = bu_im.at[:, 0].add(ab_re * h_im + ab_im * h_re)
        _, _, hs_re, hs_im = lax.associative_scan(complex_linear_combine, (a_blk_re, a_blk_im, bu_re, bu_im), axis=1)
        y = (jnp.einsum('blgp,gip->blgi', hs_re, c_re) - jnp.einsum('blgp,gip->blgi', hs_im, c_im)
             + d_g * u_blk)
        return (hs_re[:, -1], hs_im[:, -1]), y

    init = (jnp.zeros((b, S5_GROUPS, S5_STATE), F32), jnp.zeros((b, S5_GROUPS, S5_STATE), F32))
    _, ys = lax.scan(block_step, init, ub)
    return jnp.moveaxis(ys, 0, 1).reshape(b, s, w)


def setup_inputs(seed: int = 0) -> dict:
    key = jax.random.key(seed)
    ks = jax.random.split(key, 32)

    def nrm(k, shape, scale):
        return scale * jax.random.normal(k, shape, F32)

    W = BRANCH_WIDTH
    a_pow = jax.random.uniform(ks[14], (DEPTH, W), F32, 0.9, 0.999)
    p = a_pow ** (1.0 / LRU_C)
    return {
        'x': nrm(ks[0], (BATCH, SEQ, D_MODEL), 1.0),
        'norm_gain': 1.0 + nrm(ks[1], (DEPTH, D_MODEL), 0.02),
        'w_in': nrm(ks[2], (DEPTH, D_MODEL, IN_COLS), D_MODEL ** -0.5),
        'gla_w_lr': nrm(ks[3], (DEPTH, GLA_RANK, GLA_KEY), GLA_RANK ** -0.5),
        'gla_b_lr': nrm(ks[4], (DEPTH, GLA_KEY), 0.1),
        'gla_norm_gain': 1.0 + nrm(ks[5], (DEPTH, W), 0.02),
        'ret_norm_gain': 1.0 + nrm(ks[6], (DEPTH, W), 0.02),
        'ret_norm_bias': nrm(ks[7], (DEPTH, W), 0.02),
        'lru_conv_w': nrm(ks[8], (DEPTH, CONV_WIDTH, W), CONV_WIDTH ** -0.5),
        'lru_conv_b': nrm(ks[9], (DEPTH, W), 0.02),
        'lru_w_a': nrm(ks[10], (DEPTH, LRU_BLOCKS, LRU_BLOCK_DIM, LRU_BLOCK_DIM), LRU_BLOCK_DIM ** -0.5),
        'lru_b_a': nrm(ks[11], (DEPTH, W), 0.02),
        'lru_w_x': nrm(ks[12], (DEPTH, LRU_BLOCKS, LRU_BLOCK_DIM, LRU_BLOCK_DIM), LRU_BLOCK_DIM ** -0.5),
        'lru_b_x': nrm(ks[13], (DEPTH, W), 0.02),
        'lru_lambda': jnp.log(p) - jnp.log1p(-p),
        's5_a_re': -0.5 + nrm(ks[15], (DEPTH, S5_GROUPS, S5_STATE), 0.01),
        's5_a_im': jnp.pi * jnp.arange(S5_STATE, dtype=F32) + nrm(ks[16], (DEPTH, S5_GROUPS, S5_STATE), 0.01),
        's5_b_re': nrm(ks[17], (DEPTH, S5_GROUPS, S5_STATE, S5_GROUP), (2.0 * S5_GROUP) ** -0.5),
        's5_b_im': nrm(ks[18], (DEPTH, S5_GROUPS, S5_STATE, S5_GROUP), (2.0 * S5_GROUP) ** -0.5),
        's5_c_re': nrm(ks[19], (DEPTH, S5_GROUPS, S5_GROUP, S5_STATE), (2.0 * S5_STATE) ** -0.5),
        's5_c_im': nrm(ks[20], (DEPTH, S5_GROUPS, S5_GROUP, S5_STATE), (2.0 * S5_STATE) ** -0.5),
        's5_d': nrm(ks[21], (DEPTH, W), 1.0),
        's5_log_dt': jax.random.uniform(ks[22], (DEPTH, S5_GROUPS), F32, math.log(0.001), math.log(0.1)),
        's5_glu_w': nrm(ks[23], (DEPTH, W, W), W ** -0.5),
        's5_glu_b': nrm(ks[24], (DEPTH, W), 0.02),
        'w_merge_gate': nrm(ks[25], (DEPTH, N_BRANCHES, D_MODEL, D_MODEL), D_MODEL ** -0.5),
        'w_branch': nrm(ks[26], (DEPTH, N_BRANCHES, W, D_MODEL), W ** -0.5),
        'w_out': nrm(ks[27], (DEPTH, D_MODEL, D_MODEL), D_MODEL ** -0.5),
        'final_norm_gain': 1.0 + nrm(ks[28], (D_MODEL,), 0.02),
    }


def reference(x, norm_gain, w_in, gla_w_lr, gla_b_lr, gla_norm_gain, ret_norm_gain, ret_norm_bias,
              lru_conv_w, lru_conv_b, lru_w_a, lru_b_a, lru_w_x, lru_b_x, lru_lambda,
              s5_a_re, s5_a_im, s5_b_re, s5_b_im, s5_c_re, s5_c_im, s5_d, s5_log_dt, s5_glu_w, s5_glu_b,
              w_merge_gate, w_branch, w_out, final_norm_gain):
    dt = x.dtype
    h = x
    for l in range(DEPTH):
        u = rmsnorm(h, norm_gain[l])
        proj = u @ w_in[l]
        (gq, gk, gv, glr, gg, rq, rk, rv, rg, lx, lg, su, sg) = split_columns(proj)
        o_gla = gla_mix(gq, gk, gv, glr, gla_w_lr[l], gla_b_lr[l], gla_norm_gain[l]).astype(dt) * jax.nn.silu(gg)
        o_ret = retention_mix(rq, rk, rv, ret_norm_gain[l], ret_norm_bias[l]).astype(dt) * jax.nn.silu(rg)
        o_lru = rglru_mix(lx, lru_conv_w[l], lru_conv_b[l], lru_w_a[l], lru_b_a[l], lru_w_x[l], lru_b_x[l],
                          lru_lambda[l]).astype(dt) * jax.nn.silu(lg)
        y5 = jax.nn.gelu(s5_scan(su, s5_a_re[l], s5_a_im[l], s5_b_re[l], s5_b_im[l], s5_c_re[l], s5_c_im[l],
                                 s5_d[l], s5_log_dt[l])).astype(dt)
        y5 = y5 * jax.nn.sigmoid(y5 @ s5_glu_w[l] + s5_glu_b[l])
        o_s5 = y5 * jax.nn.silu(sg)
        merged = jnp.zeros_like(h)
        for bi, o in enumerate((o_gla, o_ret, o_lru, o_s5)):
            gate = jax.nn.sigmoid(u @ w_merge_gate[l, bi])
            merged = merged + gate * (o @ w_branch[l, bi])
        h = h + merged @ w_out[l]
    return rmsnorm(h, final_norm_gain)
```

```python
import math
from contextlib import ExitStack

import numpy as np
import concourse.bass as bass
import concourse.mybir as mybir
from concourse.bass_utils import run_bass_kernel_spmd

F32 = mybir.dt.float32
BF16 = mybir.dt.bfloat16
I32 = mybir.dt.int32
AF = mybir.ActivationFunctionType
ALU = mybir.AluOpType

D = 1024
KC = 8
T = 512
NTB = T // 128
NCH = T // 64
W = 512
NFT = 4
DEPTH = 4
IN_COLS = 5136
EPS = 1e-6
C_GQ, C_GK, C_GV, C_GLR, C_GG = 0, 256, 512, 1024, 1040
C_RQ, C_RK, C_RV, C_RG = 1552, 1808, 2064, 2576
C_LX, C_LG, C_SU, C_SG = 3088, 3600, 4112, 4624
NPP = 110
TWO_PI = 2.0 * math.pi
CW1 = 6.28125
CW2 = TWO_PI - CW1
GELU_C = math.sqrt(2.0 / math.pi)


class Res:
    __slots__ = ("name", "lw", "rd")

    def __init__(self, name):
        self.name = name
        self.lw = None
        self.rd = set()


class V:
    __slots__ = ("ap", "res")

    def __init__(self, ap, res):
        self.ap = ap
        self.res = tuple(res)

    def __getitem__(self, key):
        return V(self.ap[key], self.res)


class Tile:
    def __init__(self, t, name, nparts):
        self.t = t
        self.name = name
        self.res = tuple(Res(f"{name}.{i}") for i in range(max(1, nparts)))
        self.parts = nparts > 0

    def __getitem__(self, key):
        if self.parts and isinstance(key, tuple) and len(key) >= 2 and isinstance(key[1], int):
            return V(self.t[key], (self.res[key[1]],))
        return V(self.t[key], self.res)

    def all(self):
        return V(self.t[:], self.res)


ENGS = ("pe", "act", "dve", "pool", "sp")


class Node:
    __slots__ = ("id", "q", "fn", "dma", "deps", "odeps", "dur", "nbytes", "is_out", "tag", "t0", "t1")

    def __init__(self, id, q, fn, dma, deps, odeps, dur, nbytes=0):
        self.id, self.q, self.fn, self.dma = id, q, fn, dma
        self.deps, self.odeps, self.dur, self.nbytes = deps, odeps, dur, nbytes
        self.is_out = False


class Prog:
    NDS = 16
    NSW = 76
    DMA_BW = 150.0

    def __init__(self, nc, stack, reorder=True):
        self.nc = nc
        self.stack = stack
        self.nodes = []
        self.cur = ""
        self.why = None
        self.reorder = reorder
        self.esem = {e: stack.enter_context(nc.semaphore(f"s_{e}")) for e in ENGS}
        self.dsem = [stack.enter_context(nc.semaphore(f"d_{i}")) for i in range(self.NDS)]

    def sb(self, name, shape, dtype, nparts=0):
        t = self.stack.enter_context(self.nc.sbuf_tensor("sb_" + name, list(shape), dtype))
        return Tile(t, name, nparts)

    def ps(self, name, shape, dtype=F32):
        t = self.stack.enter_context(self.nc.psum_tensor("ps_" + name, list(shape), dtype))
        return Tile(t, name, 0)

    def _collect(self, q, reads, writes):
        deps, odeps = set(), set()
        nodes = self.nodes
        why = self.why
        for r in reads:
            if r.lw is not None:
                deps.add(r.lw)
                if why is not None:
                    why[(len(nodes), r.lw)] = ("RAW", r.name)
        for w in writes:
            if w.lw is not None:
                if q == "pe" and nodes[w.lw].q == "pe" and not nodes[w.lw].dma:
                    odeps.add(w.lw)
                else:
                    deps.add(w.lw)
                if why is not None:
                    why[(len(nodes), w.lw)] = ("WAW", w.name)
            deps.update(w.rd)
            if why is not None:
                for x in w.rd:
                    why[(len(nodes), x)] = ("WAR", w.name)
        return deps, odeps

    def _add(self, q, fn, dma, reads, writes, dur, nbytes=0):
        deps, odeps = self._collect(q, reads, writes)
        n = Node(len(self.nodes), q, fn, dma, deps, odeps - deps, dur, nbytes)
        n.tag = self.cur
        self.nodes.append(n)
        for r in reads:
            r.rd.add(n.id)
        for w in writes:
            w.lw = n.id
            w.rd = set()
        return n

    def op(self, eng, fn, reads, writes, dur=300.0):
        return self._add(eng, fn, False, reads, writes, dur)

    def handoff(self, old, new):
        toks = set()
        for r in old:
            if r.lw is not None:
                toks.add(r.lw)
            toks.update(r.rd)
        for r in new:
            r.lw = None
            r.rd = set(toks)

    def dma(self, q, out, in_, is_output=False, **kw):
        oap, iap = out.ap, in_.ap
        nbytes = 1
        for d in oap.shape:
            nbytes *= int(d)
        nbytes *= 4 if oap.dtype == F32 else 2
        n = self._add(q, lambda e: e.dma_start(out=oap, in_=iap, **kw), True, in_.res, out.res, 0.0, nbytes)
        n.is_out = is_output
        return n

    def _schedule(self):
        import heapq
        nodes = self.nodes
        N = len(nodes)
        order = {e: [] for e in ENGS}
        if not self.reorder:
            for n in nodes:
                order[n.q].append(n.id)
            return order
        succ = [[] for _ in range(N)]
        cnt = [0] * N
        for n in nodes:
            ds = n.deps | n.odeps
            cnt[n.id] = len(ds)
            for d in ds:
                succ[d].append(n.id)
        ready = {e: [] for e in ENGS}
        for n in nodes:
            if cnt[n.id] == 0:
                heapq.heappush(ready[n.q], n.id)
        free_at = {e: 0.0 for e in ENGS}
        busy = {e: False for e in ENGS}
        events = []
        dma_free = 0.0
        now = 0.0
        done = 0

        def dispatch(e, now):
            nonlocal dma_free
            if busy[e] or not ready[e]:
                return
            i = heapq.heappop(ready[e])
            n = nodes[i]
            order[e].append(i)
            busy[e] = True
            n.t0 = now
            if n.dma:
                issue = 1500.0 if e == "pool" else 100.0
                t0 = max(now + issue, dma_free)
                dma_free = t0 + n.nbytes / self.DMA_BW
                n.t1 = dma_free + 2000.0
                heapq.heappush(events, (now + issue, 0, e_idx[e]))
                heapq.heappush(events, (dma_free + 2000.0, 1, i))
            else:
                n.t1 = now + n.dur + 60.0
                heapq.heappush(events, (now + n.dur, 0, e_idx[e]))
                heapq.heappush(events, (now + n.dur + 60.0, 1, i))

        e_idx = {e: k for k, e in enumerate(ENGS)}
        for e in ENGS:
            dispatch(e, 0.0)
        while events:
            now, kind, x = heapq.heappop(events)
            if kind == 0:
                e = ENGS[x]
                busy[e] = False
                dispatch(e, now)
            else:
                done += 1
                for s_ in succ[x]:
                    cnt[s_] -= 1
                    if cnt[s_] == 0:
                        q = nodes[s_].q
                        heapq.heappush(ready[q], s_)
                        dispatch(q, now)
        assert done == N, (done, N)
        self.sim_ns = now
        return order

    def emit(self):
        nc = self.nc
        nodes = self.nodes
        order = self._schedule()
        tok = [None] * len(nodes)
        sems = {("e", e): self.esem[e] for e in ENGS}
        for i in range(self.NDS):
            sems[("d", i)] = self.dsem[i]
        extra = {}
        nsw = 0
        dcnt = [0] * self.NDS
        dnext = 0
        for e in ENGS:
            pos = 0
            for i in order[e]:
                n = nodes[i]
                if not n.dma:
                    pos += 1
                    tok[i] = (("e", e), pos)
                elif e == "pool":
                    assert nsw < self.NSW, "out of one-shot semaphores"
                    key = ("w", nsw)
                    sems[key] = self.stack.enter_context(nc.semaphore(f"w_{nsw}"))
                    nsw += 1
                    tok[i] = (key, 16)
                else:
                    k = dnext
                    dnext = (dnext + 1) % self.NDS
                    if dcnt[k]:
                        extra[i] = (("d", k), dcnt[k])
                    dcnt[k] += 16
                    tok[i] = (("d", k), dcnt[k])
        fin = {}
        for i, n in enumerate(nodes):
            k, v = tok[i]
            if fin.get(k, 0) < v:
                fin[k] = v
        block = self.stack.enter_context(nc.Block())

        def replay(eng_handle, e):
            seen = {}
            for i in order[e]:
                n = nodes[i]
                need = {}
                for d in n.deps:
                    k, v = tok[d]
                    if need.get(k, 0) < v:
                        need[k] = v
                if i in extra:
                    k, v = extra[i]
                    if need.get(k, 0) < v:
                        need[k] = v
                for k, v in need.items():
                    if seen.get(k, 0) < v:
                        seen[k] = v
                        eng_handle.wait_ge(sems[k], v)
                k, v = tok[i]
                n.fn(eng_handle).then_inc(sems[k], 16 if n.dma else 1)
            if e == "sp":
                for k, v in fin.items():
                    if seen.get(k, 0) < v:
                        eng_handle.wait_ge(sems[k], v)

        @block.tensor
        def _(e):
            replay(e, "pe")

        @block.scalar
        def _(e):
            replay(e, "act")

        @block.vector
        def _(e):
            replay(e, "dve")

        @block.gpsimd
        def _(e):
            replay(e, "pool")

        @block.sync
        def _(e):
            replay(e, "sp")

    def finish(self):
        pass

    @staticmethod
    def _sc(x, reads):
        if isinstance(x, V):
            reads.extend(x.res)
            return x.ap
        return x

    @staticmethod
    def _n(v):
        n = 1
        for d in v.ap.shape[1:]:
            n *= int(d)
        return n

    def act(self, out, in_, func, bias=0.0, scale=1.0):
        reads = list(in_.res)
        b = self._sc(bias, reads)
        s = self._sc(scale, reads)
        o, i = out.ap, in_.ap
        return self.op("act", lambda e: e.activation(out=o, in_=i, func=func, bias=b, scale=s), reads, out.res,
                       dur=220.0 + 0.85 * self._n(in_))

    def tt(self, eng, out, in0, in1, op):
        o, a, b = out.ap, in0.ap, in1.ap
        n = self._n(in0)
        dur = (100.0 + 1.05 * n) if eng == "dve" else (150.0 + 2.6 * n)
        return self.op(eng, lambda e: e.tensor_tensor(out=o, in0=a, in1=b, op=op), in0.res + in1.res, out.res, dur=dur)

    def ts(self, eng, out, in0, s1, s2, op0, op1=None):
        reads = list(in0.res)
        a1 = self._sc(s1, reads)
        a2 = self._sc(s2, reads)
        o, a = out.ap, in0.ap
        n = self._n(in0)
        dur = (100.0 + 1.05 * n) if eng == "dve" else (150.0 + 2.6 * n)
        if op1 is None:
            return self.op(eng, lambda e: e.tensor_scalar(out=o, in0=a, scalar1=a1, scalar2=None, op0=op0), reads, out.res, dur=dur)
        return self.op(eng, lambda e: e.tensor_scalar(out=o, in0=a, scalar1=a1, scalar2=a2, op0=op0, op1=op1), reads, out.res, dur=dur)

    def stt(self, out, in0, scalar, in1, op0, op1):
        reads = list(in0.res) + list(in1.res)
        s = self._sc(scalar, reads)
        o, a, b = out.ap, in0.ap, in1.ap
        return self.op("dve", lambda e: e.scalar_tensor_tensor(out=o, in0=a, scalar=s, in1=b, op0=op0, op1=op1), reads, out.res,
                       dur=100.0 + 1.05 * self._n(in0))

    def scan(self, out, d0, d1, init):
        reads = list(d0.res) + list(d1.res)
        ini = self._sc(init, reads)
        o, a, b = out.ap, d0.ap, d1.ap
        return self.op("dve", lambda e: e.tensor_tensor_scan(out=o, data0=a, data1=b, initial=ini, op0=ALU.mult, op1=ALU.add), reads, out.res,
                       dur=100.0 + 2.1 * self._n(d1))

    def copy(self, eng, out, in_):
        o, i = out.ap, in_.ap
        n = self._n(in_)
        if eng == "act":
            return self.op("act", lambda e: e.activation(out=o, in_=i, func=AF.Copy), in_.res, out.res, dur=220.0 + 0.85 * n)
        dur = (100.0 + 1.05 * n) if eng == "dve" else (150.0 + 2.6 * n)
        return self.op(eng, lambda e: e.tensor_copy(out=o, in_=i), in_.res, out.res, dur=dur)

    def recip(self, out, in_):
        o, i = out.ap, in_.ap
        return self.op("dve", lambda e: e.reciprocal(out=o, in_=i), in_.res, out.res, dur=100.0 + 6.5 * self._n(in_))

    def memset(self, eng, out, val):
        o = out.ap
        return self.op(eng, lambda e: e.memset(o, val), (), out.res, dur=100.0 + 0.5 * self._n(out))

    def mm(self, out, lhsT, rhs, start, stop):
        o, l, r = out.ap, lhsT.ap, rhs.ap
        return self.op("pe", lambda e: e.matmul(o, l, r, start=start, stop=stop), lhsT.res + rhs.res, out.res,
                       dur=40.0 + 0.45 * max(64, self._n(rhs)))

    def tr(self, out, in_, ident):
        o, i, d = out.ap, in_.ap, ident.ap
        return self.op("pe", lambda e: e.transpose(o, i, d), in_.res + ident.res, out.res, dur=100.0)


def _pack_params(inp, layers):
    L = len(layers)
    pp = np.zeros((L, 128, NPP), np.float32)
    lruw = np.zeros((L, 128, 2, 4, 128), np.float32)
    cT = np.zeros((L, 128, 2, 16, 128), np.float32)
    bT = np.zeros((L, 2, 128, 4, 2, 128), np.float32)
    aT = np.zeros((L, 3, 128, 4, 2, 128), np.float32)
    wlr = np.zeros((L, 16, 256), np.float32)
    for li, l in enumerate(layers):
        g = lambda nm: np.asarray(inp[nm][l], np.float32)
        pp[li, :, 0:8] = g("norm_gain").reshape(8, 128).T
        pp[li, :, 8:10] = g("gla_b_lr").reshape(2, 128).T
        pp[li, :, 10:14] = g("gla_norm_gain").reshape(4, 128).T
        pp[li, :, 14:18] = g("ret_norm_gain").reshape(4, 128).T
        pp[li, :, 18:22] = g("ret_norm_bias").reshape(4, 128).T
        cw = g("lru_conv_w")
        pp[li, :, 22:38] = cw.reshape(4, 4, 128).transpose(2, 1, 0).reshape(128, 16)
        pp[li, :, 38:42] = g("lru_conv_b").reshape(4, 128).T
        pp[li, :, 42:46] = g("lru_b_a").reshape(4, 128).T
        pp[li, :, 46:50] = g("lru_b_x").reshape(4, 128).T
        pp[li, :, 50:54] = g("lru_lambda").reshape(4, 128).T
        pp[li, :, 54:58] = g("s5_d").reshape(4, 128).T
        pp[li, :, 58:62] = g("s5_glu_b").reshape(4, 128).T
        are = g("s5_a_re").reshape(16, 2, 64)
        aim = g("s5_a_im").reshape(16, 2, 64)
        ldt = g("s5_log_dt").reshape(16, 2)
        pp[li, :, 62:78] = are.transpose(1, 2, 0).reshape(128, 16)
        pp[li, :, 78:94] = aim.transpose(1, 2, 0).reshape(128, 16)
        pp[li, :, 94:110] = np.repeat(ldt.T[:, None, :], 64, axis=1).reshape(128, 16)
        ldt_b = np.repeat(ldt[:, :, None], 64, axis=2)
        for q2 in range(2):
            for ft in range(4):
                for e in range(2):
                    gp = 4 * ft + 2 * q2 + e
                    aT[li, 0, q2 * 64:(q2 + 1) * 64, ft, e, :] = are[gp].reshape(1, 128)
                    aT[li, 1, q2 * 64:(q2 + 1) * 64, ft, e, :] = aim[gp].reshape(1, 128)
                    aT[li, 2, q2 * 64:(q2 + 1) * 64, ft, e, :] = ldt_b[gp].reshape(1, 128)
        wlr[li] = g("gla_w_lr")
        for ax, nm in enumerate(("lru_w_a", "lru_w_x")):
            wv = g(nm)
            for blk in range(8):
                h2 = blk % 2
                lruw[li, h2 * 64:(h2 + 1) * 64, ax, blk // 2, h2 * 64:(h2 + 1) * 64] = wv[blk]
        for ri, nm in enumerate(("s5_c_re", "s5_c_im")):
            cv = g(nm).reshape(16, 2, 16, 64)
            for two in range(2):
                for gp in range(16):
                    c0 = (gp % 4) * 32 + two * 16
                    cT[li, two * 64:(two + 1) * 64, ri, gp, c0:c0 + 16] = cv[gp, two].T
        for ri, nm in enumerate(("s5_b_re", "s5_b_im")):
            bv = g(nm).reshape(16, 2, 64, 16)
            for two in range(2):
                for q2 in range(2):
                    for ft in range(4):
                        for e in range(2):
                            gp = 4 * ft + 2 * q2 + e
                            r0 = q2 * 64 + e * 32 + two * 16
                            bT[li, ri, r0:r0 + 16, ft, e, two * 64:(two + 1) * 64] = bv[gp, two].T
    return dict(pp=pp, lruw=lruw, s5cT=cT, s5bT=bT.reshape(L, 2, 128, 1024),
                s5aT=aT.reshape(L, 3, 128, 1024), wlr=wlr)


def _consts():
    c = np.zeros((128, 1216), np.float32)
    j = np.arange(128)[:, None]
    i = np.arange(128)[None, :]
    c[:, 0:128] = ((j // 64 == i // 64) & (i >= j)).astype(np.float32)
    t = np.arange(T)
    c[:, 128:640] = (t % 64 != 0).astype(np.float32)[None, :]
    c[:, 640:1152] = ((t % 64) + 1).astype(np.float32)[None, :]
    p = np.arange(128)
    ii = p % 32
    c[:, 1152] = (10000.0 ** (-(ii.astype(np.float64)) / 32.0)).astype(np.float32)
    for tile in range(2):
        hh = 2 * tile + p // 64
        c[:, 1153 + tile] = np.log1p(-np.exp2(-5.0 - hh.astype(np.float64))).astype(np.float32)
    c[:, 1155] = np.where((p % 64) < 32, -1.0, 1.0)
    idn = np.eye(128, dtype=np.float32)
    tpos = np.arange(T, dtype=np.float32)[None, :].repeat(128, 0)
    return c, idn, tpos


GLA_SEGS = [[(h // 2, (h % 2) * 64, 64)] for h in range(4)]
RET_SEGS = [[(0, 32 * h, 32), (1, 32 * h, 32)] for h in range(4)]


class Builder:
    def __init__(self, n_layers, n_seq, tiles_per_seq, final_norm=True, debug=None, mixers=(0, 1, 2, 3), stage=99):
        self.L = n_layers
        self.n_seq = n_seq
        self.tps = tiles_per_seq
        self.final_norm = final_norm
        self.debug = debug or []
        self.mixers = mixers
        self.stage = stage
        self.S = tiles_per_seq * T

    def build(self):
        L = self.L
        nc = bass.Bass("TRN2", target_bir_lowering=False)
        self.nc = nc
        S = self.S
        dr = {}

        def din(name, shape, dt=F32):
            dr[name] = nc.dram_tensor(name, list(shape), dt, kind="ExternalInput").ap()
            return dr[name]

        din("xT", [self.n_seq, D, S])
        din("w_in", [L, D, IN_COLS])
        din("w_mg", [L, 4, D, D])
        din("w_br", [L, 4, W, D])
        din("w_out", [L, D, D])
        din("w_glu", [L, W, W])
        din("pp", [L, 128, NPP])
        din("fg", [128, 8])
        din("lruw", [L, 128, 2, 4, 128])
        din("s5cT", [L, 128, 2, 16, 128])
        din("s5bT", [L, 2, 128, 1024])
        din("s5aT", [L, 3, 128, 1024])
        din("wlr", [L, 16, 256])
        din("cst", [128, 1216])
        din("idn", [128, 128])
        din("tpos", [128, T])
        self.yT = nc.dram_tensor("yT", [self.n_seq, D, S], F32, kind="ExternalOutput").ap()
        self.bbscr = nc.dram_tensor("bbscr", [L, 128, 2, 1024], BF16, kind="Internal").ap()
        self.rotscr = nc.dram_tensor("rotscr", [L, 16, 128, 2, T], F32, kind="Internal").ap()
        self.ddscr = nc.dram_tensor("ddscr", [L, 128, 512], BF16, kind="Internal").ap()
        self.ddres = [Res(f"ddscr{l}") for l in range(L)]
        def scr(name, shape):
            return nc.dram_tensor(name, list(shape), BF16, kind="Internal").ap()
        self.sc = dict(w_in=scr("sc_in", [L, D, IN_COLS]), w_mg=scr("sc_mg", [L, 4, D, D]), w_br=scr("sc_br", [L, 4, W, D]),
                       w_out=scr("sc_out", [L, D, D]), w_glu=scr("sc_glu", [L, W, W]),
                       rsw=scr("sc_rsw", [L, D, 512]),
                       lruw=scr("sc_lruw", [L, 128, 1024]), s5cT=scr("sc_cT", [L, 128, 4096]),
                       wlr=scr("sc_wlr", [L, 16, 256]), idn=scr("sc_idn", [128, 128]))
        self.scres = {k: [Res(f"sc_{k}{l}") for l in range(L)] for k in self.sc}
        self.bbres = [Res(f"bbscr{l}") for l in range(L)]
        self.rotres = [[Res(f"rotscr{l}_{g}") for g in range(16)] for l in range(L)]
        self.dbg_out = {}
        for name, shape in self.debug:
            self.dbg_out[name] = nc.dram_tensor("dbg_" + name, list(shape), F32, kind="ExternalOutput").ap()
        self.dr = dr

        with ExitStack() as stack:
            P = Prog(nc, stack)
            self.P = P
            self.alloc()
            if self.stage >= 1:
                self.convert_weights()
            self.plan_weights()
            if self.stage >= 2:
                self.prologue()
            for sq in range(self.n_seq):
                self.reset_state()
                for ti in range(self.tps):
                    self.tile_step(sq, ti)
            P.finish()
            P.emit()
        return nc

    def alloc(self):
        P = self.P
        L = self.L
        sb = P.sb
        self.h = sb("h", [128, KC, T], F32, KC)
        self.u = sb("u", [128, KC, T], BF16, KC)
        self.mg = sb("mg", [128, KC, T], F32, KC)
        self.NS = 4
        self.SLOT = 8 * 528
        self.ring = [sb(f"ring{i}", [128, self.SLOT], BF16) for i in range(self.NS)]
        self.cst = sb("cst", [128, 1216], F32)
        self.idn = sb("idn", [128, 128], BF16)
        self.ones = sb("ones", [128, 128], BF16)
        self.tpos = sb("tpos", [128, T], F32)
        self.pp = sb("pp", [128, L, NPP], F32)
        self.fg = sb("fg", [128, 8], F32)
        self.lruw = sb("lruw", [128, 2, 4, 128], BF16)
        self.cTw = sb("cTw", [128, 2, 16, 128], BF16)
        self.wlr = sb("wlr", [16, L, 256], BF16)
        self.nblr = sb("nblr", [128, L, 2], F32)
        self.lsc = sb("lsc", [128, L, 4], F32)
        self.rho = sb("rho", [128, L, 16], F32)
        self.g64 = sb("g64", [128, 4], F32)
        self.rt = sb("rt", [128, 2, 4, T], F32, 2)
        self.f = [sb(f"f{i}", [128, T], F32) for i in range(10)]
        self.b = [sb(f"b{i}", [128, T], BF16) for i in range(8)]
        self.sg = sb("sg", [128, NFT, T], F32, NFT)
        self.obr_all = sb("obr", [128, 4 * NFT, T], BF16, 4 * NFT)
        self.obr = None
        self.mgt = [sb(f"mgt{i}", [128, T], F32) for i in range(3)]
        self.itmp = Tile(self.mgt[2].t[:].bitcast(I32), "itmp", 0)
        self.itmp.res = self.mgt[2].res
        self.epsb = sb("epsb", [128, 1], F32)
        self.oneb = sb("oneb", [128, 1], F32)
        self.l8b = sb("l8b", [128, 1], F32)
        MXB = 14848
        self.mx = sb("mx", [128, MXB], BF16)
        raw = self.mx.t

        def view(off_bf16, shape, dt, name, nparts=0):
            n = int(np.prod(shape[1:]))
            nb16 = n * (2 if dt == F32 else 1)
            ap = raw[:, off_bf16:off_bf16 + nb16]
            if dt == F32:
                ap = ap.bitcast(F32)
            if len(shape) == 3:
                ap = ap.rearrange("p (a b) -> p a b", a=shape[1])
            elif len(shape) == 4:
                ap = ap.rearrange("p (a b c) -> p a b c", a=shape[1], b=shape[2])
            return Tile(ap, name, nparts), off_bf16 + nb16

        o = 0
        self.vtok, o = view(o, [128, NTB, 512], BF16, "vtok", NTB)
        self.kttok, o = view(o, [128, NTB, 256], BF16, "kttok", NTB)
        self.sT, o = view(o, [128, NTB, 4, 128], BF16, "sT", NTB)
        self.S32, o = view(o, [128, 2, NCH + 1, 128], F32, "S32", 2)
        self.Sbf, o = view(o, [128, 2, NCH, 128], BF16, "Sbf", 2)
        assert o <= MXB, o
        self.la_res = self.vtok.res + self.kttok.res + self.sT.res + self.S32.res + self.Sbf.res
        o = 0
        self.lxh, o = view(o, [128, NFT, T + 4], F32, "lxh", NFT)
        self.lru_res = self.lxh.res
        o = 0
        self.bbw, o = view(o, [128, 2, 8, 128], BF16, "bbw")
        self.rot0, o = view(o, [128, 2, T], F32, "rot0")
        self.rot1, o = view(o, [128, 2, T], F32, "rot1")
        self.rot = [self.rot0, self.rot1]
        self.Hs, o = view(o, [128, 2, 4, T], BF16, "Hs", 2)
        self.y5, o = view(o, [128, NFT, T], F32, "y5", NFT)
        self.dD, o = view(o, [128, NFT, 128], BF16, "dD")
        assert o <= MXB, o
        self.s5_res = self.bbw.res + self.rot0.res + self.rot1.res + self.Hs.res + self.y5.res + self.dD.res
        self.mx_cur = None
        self.st_gla = sb("st_gla", [128, L, 2, 128], F32, L)
        self.st_ret = sb("st_ret", [128, L, 2, 128], F32, L)
        self.st_lru = sb("st_lru", [128, L, 4], F32, L)
        self.st_lxh = sb("st_lxh", [128, L, 4, 4], F32, L)
        self.st_s5 = sb("st_s5", [128, L, 2, 16], F32, L)
        self.bank = [P.ps(f"bank{i}", [128, T], F32) for i in range(8)]
        self.bk = 0

    def mx_switch(self, which):
        groups = {"la": self.la_res, "lru": self.lru_res, "s5": self.s5_res}
        if self.mx_cur is not None:
            self.P.handoff(groups[self.mx_cur], groups[which])
        self.mx_cur = which

    def nb(self, pool=None):
        if pool is not None:
            ids = pool[0]
            b = self.bank[ids[pool[1] % len(ids)]]
            pool[1] += 1
            return b
        b = self.bank[self.bk]
        self.bk = (self.bk + 1) % 8
        return b

    def convert_weights(self):
        P = self.P
        dr = self.dr

        def flat(ap, pat, n=2048):
            return ap.rearrange(pat).rearrange("(a b) -> a b", b=n)
        for l in range(self.L):
            for k, pat, n in (("w_in", "a b -> (a b)", 2048), ("w_mg", "g a b -> (g a b)", 2048), ("w_br", "g a b -> (g a b)", 2048),
                              ("w_out", "a b -> (a b)", 2048), ("w_glu", "a b -> (a b)", 2048)):
                P.dma("pool", V(flat(self.sc[k][l], pat, n), (self.scres[k][l],)), V(flat(dr[k][l], pat, n), ()))
        if 1 in self.mixers:
            for l in range(self.L):
                src = dr["w_in"][l][:, C_RQ:C_RQ + 512].rearrange("r (g two i) -> r g two i", two=2, i=32)
                dst = self.sc["rsw"][l].rearrange("r (g two i) -> r g two i", two=2, i=32)
                for two in range(2):
                    P.dma("pool", V(dst[:, :, two, :], (self.scres["rsw"][l],)), V(src[:, :, 1 - two, :], ()))
        P.dma("pool", V(self.sc["lruw"].rearrange("l p n -> (l p) n"), tuple(self.scres["lruw"])),
              V(dr["lruw"].rearrange("l p a f n -> (l p) (a f n)"), ()))
        P.dma("pool", V(self.sc["s5cT"].rearrange("l p n -> (l p) n"), tuple(self.scres["s5cT"])),
              V(dr["s5cT"].rearrange("l p a g n -> (l p) (a g n)"), ()))
        P.dma("pool", V(self.sc["wlr"].rearrange("l r n -> (l r) n"), tuple(self.scres["wlr"])),
              V(dr["wlr"].rearrange("l r n -> (l r) n"), ()))
        P.dma("pool", V(self.sc["idn"], tuple(self.scres["idn"])), V(dr["idn"], ()))

    def plan_weights(self):
        dr = self.sc
        rs = self.scres
        self.wplan = []
        for sq in range(self.n_seq):
            for ti in range(self.tps):
                for l in range(self.L):
                    def win(c0, n):
                        return ("in", V(dr["w_in"][l].rearrange("(k p) n -> p k n", p=128)[:, :, c0:c0 + n], (rs["w_in"][l],)), KC, n)

                    def mgs(b):
                        v = dr["w_mg"][l, b].rearrange("(k p) n -> p k n", p=128)
                        br = dr["w_br"][l, b].rearrange("(k p) n -> p k n", p=128)
                        rm, rb_ = (rs["w_mg"][l],), (rs["w_br"][l],)
                        return [("mg", V(v[:, :, 0:512], rm), KC, 512), ("br", V(br[:, :, 0:512], rb_), 4, 512),
                                ("mg", V(v[:, :, 512:1024], rm), KC, 512), ("br", V(br[:, :, 512:1024], rb_), 4, 512)]
                    g = []
                    if 0 in self.mixers:
                        g += [win(C_GLR, 528), win(C_GQ, 512), win(C_GV, 512)]
                    if 1 in self.mixers:
                        g += [win(C_RQ, 512), ("in", V(dr["rsw"][l].rearrange("(k p) n -> p k n", p=128), (rs["rsw"][l],)), KC, 512),
                              win(C_RV, 512), win(C_RG, 512)]
                    if 2 in self.mixers:
                        g += [win(C_LX, 512), win(C_LG, 512)]
                    if 3 in self.mixers:
                        g += [win(C_SU, 512), win(C_SG, 512),
                              ("glu", V(dr["w_glu"][l].rearrange("(k p) n -> p k n", p=128), (rs["w_glu"][l],)), 4, 512)]
                    for b_ in range(4):
                        if b_ in self.mixers:
                            g += mgs(b_)
                    wo = dr["w_out"][l].rearrange("(k p) n -> p k n", p=128)
                    ro = (rs["w_out"][l],)
                    g += [("out", V(wo[:, :, 0:512], ro), KC, 512), ("out", V(wo[:, :, 512:1024], ro), KC, 512)]
                    self.wplan += g
        self.wissued = 0
        self.wused = 0
        self.wfree = 0

    def _wissue(self):
        P = self.P
        lim = min(len(self.wplan), self.wfree + self.NS)
        while self.wissued < lim:
            n = self.wissued
            k, src, nk, ncol = self.wplan[n]
            slot = self.ring[n % self.NS]
            dst = slot.t[:, 0:nk * ncol].rearrange("p (k n) -> p k n", k=nk)
            P.dma("sp", V(dst, slot.res), src)
            self.wissued += 1

    def wnext(self, kind):
        assert self.wused - self.wfree < self.NS
        self._wissue()
        n = self.wused
        k, src, nk, ncol = self.wplan[n]
        assert k == kind, (k, kind)
        assert n < self.wissued
        slot = self.ring[n % self.NS]
        self.wused += 1
        return Tile(slot.t[:, 0:nk * ncol].rearrange("p (k n) -> p k n", k=nk), slot.name, 0), slot.res

    def wdone(self, n=1):
        self.wfree += n
        assert self.wfree <= self.wused
        self._wissue()

    def dbg(self, name, v, idx=None):
        if name in self.dbg_out:
            o = self.dbg_out[name]
            if idx is not None:
                o = o[idx]
            self.P.dma("sp" if v.ap.dtype == F32 else "pool", V(o, ()), v, is_output=True)

    def sin_of(self, x, tmp, out_sin):
        P = self.P
        it = self.itmp.all()
        P.ts("dve", it, x, 1.0 / TWO_PI, None, ALU.mult)
        P.copy("dve", tmp, it)
        P.stt(x, tmp, -CW1, x, ALU.mult, ALU.add)
        P.stt(x, tmp, -CW2, x, ALU.mult, ALU.add)
        P.ts("dve", x, x, math.pi, -math.pi, ALU.min, ALU.max)
        P.act(out_sin, x, AF.Sin)

    def proj_fm(self, ps, wt, wres, c0, ncol, rhs, nk=KC):
        for k in range(nk):
            self.P.mm(ps, V(wt.t[:, k, c0:c0 + ncol], wres), rhs[:, k, :], start=(k == 0), stop=(k == nk - 1))

    def prologue(self):
        P = self.P
        L = self.L
        dr = self.dr
        P.dma("sp", self.cst.all(), V(dr["cst"], ()))
        P.dma("sp", self.tpos.all(), V(dr["tpos"], ()))
        P.dma("sp", self.fg.all(), V(dr["fg"], ()))
        P.dma("sp", self.pp.all(), V(dr["pp"].rearrange("l p n -> p l n"), ()))
        P.dma("sp", self.idn.all(), V(self.sc["idn"], tuple(self.scres["idn"])))
        P.dma("sp", self.wlr.all(), V(self.sc["wlr"].rearrange("l r n -> r l n"), tuple(self.scres["wlr"])))
        P.memset("dve", self.ones.all(), 1.0)
        P.memset("dve", self.epsb.all(), EPS)
        P.memset("dve", self.oneb.all(), 1.0)
        P.memset("dve", self.l8b.all(), math.log(0.125))
        f = self.f
        for l in range(L):
            P.ts("dve", self.nblr[:, l, :], self.pp[:, l, 8:10], -1.0, None, ALU.mult)
            t4 = f[0][:, 0:4]
            P.act(t4, self.pp[:, l, 50:54], AF.Exp, scale=-1.0)
            P.act(t4, t4, AF.Ln, bias=self.oneb[:, 0:1])
            P.ts("dve", self.lsc[:, l, :], t4, -8.0, None, ALU.mult)
        for tile in range(2):
            lg = self.cst[:, 1153 + tile:1154 + tile]
            P.act(self.g64[:, tile:tile + 1], lg, AF.Exp, scale=64.0)
            P.ts("dve", self.g64[:, 2 + tile:3 + tile], lg, -1.0, None, ALU.mult)
            pass
        if 3 in self.mixers:
            self.mx_cur = "s5"
            self.s5_prep(0)
        self.prepped = 1

    def s5_prep(self, l):
        P = self.P
        dr = self.dr
        f = self.f
        step = f[0][:, 0:16]
        theta = f[0][:, 16:32]
        P.act(step, self.pp[:, l, 94:110], AF.Exp)
        P.tt("dve", f[0][:, 32:48], step, self.pp[:, l, 62:78], ALU.mult)
        P.act(self.rho[:, l, :], f[0][:, 32:48], AF.Exp)
        P.tt("dve", theta, step, self.pp[:, l, 78:94], ALU.mult)
        for gp in range(16):
            ang, ang2, tmp = f[1], f[2], f[3]
            rb = self.rot[gp % 2]
            P.ts("dve", ang.all(), self.tpos.all(), 1.0, theta[:, gp:gp + 1], ALU.add, ALU.mult)
            P.ts("dve", ang2.all(), ang.all(), math.pi / 2.0, None, ALU.add)
            self.sin_of(ang.all(), tmp.all(), rb[:, 1, :])
            self.sin_of(ang2.all(), tmp.all(), rb[:, 0, :])
            P.dma("sp", V(self.rotscr[l, gp], (self.rotres[l][gp],)), rb.all())
        bbv = self.bbw
        for e in range(2):
            def half(src):
                return V(src.rearrange("p (a e n) -> p a e n", a=4, e=2)[:, :, e, :], ())

            def t3(v):
                return V(v.ap.rearrange("p (a n) -> p a n", a=4), v.res)
            are, aim, stp, bre, bim = (f[k].all() for k in range(4, 9))
            P.dma("sp", t3(are), half(dr["s5aT"][l, 0]))
            P.dma("sp", t3(aim), half(dr["s5aT"][l, 1]))
            P.dma("sp", t3(stp), half(dr["s5aT"][l, 2]))
            P.dma("sp", t3(bre), half(dr["s5bT"][l, 0]))
            P.dma("sp", t3(bim), half(dr["s5bT"][l, 1]))
            t0, t1, t2, t3_, t4 = f[0].all(), f[1].all(), f[2].all(), f[3].all(), f[9].all()
            P.act(stp, stp, AF.Exp)
            P.tt("dve", t0, stp, are, ALU.mult)
            P.act(t0, t0, AF.Exp)
            P.tt("dve", t1, stp, aim, ALU.mult)
            P.ts("dve", t2, t1, math.pi / 2.0, None, ALU.add)
            self.sin_of(t1, t3_, t1)
            self.sin_of(t2, t3_, t2)
            P.tt("dve", t1, t1, t0, ALU.mult)
            P.tt("dve", t2, t2, t0, ALU.mult)
            P.ts("dve", t2, t2, -1.0, None, ALU.add)
            P.tt("dve", t0, are, are, ALU.mult)
            P.tt("dve", t3_, aim, aim, ALU.mult)
            P.tt("dve", t0, t0, t3_, ALU.add)
            P.recip(t0, t0)
            P.tt("dve", t3_, t2, are, ALU.mult)
            P.tt("dve", t4, t1, aim, ALU.mult)
            P.tt("dve", t3_, t3_, t4, ALU.add)
            P.tt("dve", t3_, t3_, t0, ALU.mult)
            P.tt("dve", t4, t1, are, ALU.mult)
            P.tt("dve", t2, t2, aim, ALU.mult)
            P.tt("dve", t4, t4, t2, ALU.subtract)
            P.tt("dve", t4, t4, t0, ALU.mult)
            P.tt("dve", t0, t3_, bre, ALU.mult)
            P.tt("dve", t1, t4, bim, ALU.mult)
            P.tt("dve", t0, t0, t1, ALU.subtract)
            P.tt("dve", t1, t3_, bim, ALU.mult)
            P.tt("dve", t2, t4, bre, ALU.mult)
            P.tt("dve", t1, t1, t2, ALU.add)
            bb4 = bbv.t[:].rearrange("p r (a e) n -> p r a e n", e=2)
            P.copy("act", V(bb4[:, 0, :, e, :], bbv.res), V(t0.ap.rearrange("p (a n) -> p a n", a=4), t0.res))
            P.copy("act", V(bb4[:, 1, :, e, :], bbv.res), V(t1.ap.rearrange("p (a n) -> p a n", a=4), t1.res))
        P.dma("sp", V(self.bbscr[l], (self.bbres[l],)), V(bbv.t[:].rearrange("p r a n -> p r (a n)"), bbv.res))
        for ft in range(NFT):
            P.ts("dve", self.dD[:, ft, :], self.idn.all(), self.pp[:, l, 54 + ft:55 + ft], None, ALU.mult)
        P.dma("sp", V(self.ddscr[l], (self.ddres[l],)), V(self.dD.t[:].rearrange("p a n -> p (a n)"), self.dD.res))

    def reset_state(self):
        P = self.P
        for t in (self.st_gla, self.st_ret, self.st_lru, self.st_lxh, self.st_s5):
            P.memset("pool", t.all(), 0.0)

    def tile_step(self, sq, ti):
        P = self.P
        t0 = ti * T
        xsrc = self.dr["xT"][sq].rearrange("(k p) s -> p k s", p=128)[:, :, t0:t0 + T]
        P.dma("sp", self.h.all(), V(xsrc, ()))
        if 1 in self.mixers:
            self.ret_tables(t0)
        for l in range(self.L):
            if self.stage >= 3:
                if 3 in self.mixers and self.prepped <= l + 1 < self.L:
                    self.P.cur = "prep"
                    self.mx_switch("s5")
                    self.s5_prep(l + 1)
                    self.prepped = l + 2
                self.layer(l)
        ydst = self.yT[sq].rearrange("(k p) s -> p k s", p=128)[:, :, t0:t0 + T]
        if self.final_norm:
            rstd = self.rms_rstd(self.h, D)
            for k in range(KC):
                P.stt(self.mg[:, k, :], self.h[:, k, :], self.fg[:, k:k + 1], rstd, ALU.mult, ALU.mult)
            P.dma("sp", V(ydst, ()), self.mg.all(), is_output=True)
        else:
            P.dma("sp", V(ydst, ()), self.h.all(), is_output=True)

    def ret_tables(self, t0):
        P = self.P
        f = self.f
        ang, ang2, tmp, sn, cs = f[0].all(), f[1].all(), f[2].all(), f[3].all(), f[4].all()
        P.ts("dve", ang, self.tpos.all(), float(t0), self.cst[:, 1152:1153], ALU.add, ALU.mult)
        P.ts("dve", ang2, ang, math.pi / 2.0, None, ALU.add)
        self.sin_of(ang, tmp, sn)
        self.sin_of(ang2, tmp, cs)
        P.ts("dve", sn, sn, self.cst[:, 1155:1156], None, ALU.mult)
        for tile in range(2):
            dqq, dqk = f[5].all(), f[6].all()
            P.act(dqq, self.cst[:, 640:1152], AF.Exp, scale=self.cst[:, 1153 + tile:1154 + tile])
            P.act(dqk, self.cst[:, 640:1152], AF.Exp, scale=self.g64[:, 2 + tile:3 + tile], bias=self.l8b[:, 0:1])
            P.tt("dve", self.rt[:, tile, 0, :], cs, dqq, ALU.mult)
            P.tt("dve", self.rt[:, tile, 1, :], sn, dqq, ALU.mult)
            P.tt("dve", self.rt[:, tile, 2, :], cs, dqk, ALU.mult)
            P.tt("dve", self.rt[:, tile, 3, :], sn, dqk, ALU.mult)

    def rms_rstd(self, src, n):
        P = self.P
        ps = self.nb()
        for k in range(KC):
            sq = self.b[k % 2]
            P.act(sq.all(), src[:, k, :], AF.Square)
            P.mm(ps.all(), self.ones.all(), sq.all(), start=(k == 0), stop=(k == KC - 1))
        r = self.f[9].all()
        P.act(r, ps.all(), AF.Ln, bias=self.epsb[:, 0:1], scale=1.0 / n)
        P.act(r, r, AF.Exp, scale=-0.5)
        return r

    def layer(self, l):
        P = self.P
        self.bk = 0
        P.cur = "norm"
        rstd = self.rms_rstd(self.h, D)
        for k in range(KC):
            P.stt(self.u[:, k, :], self.h[:, k, :], self.pp[:, l, k:k + 1], rstd, ALU.mult, ALU.mult)
        self.first_branch = True
        self.dbg("u", self.u.all())
        if self.stage < 4:
            return
        for b, name, fn in ((0, "gla", self.gla), (1, "ret", self.ret), (2, "lru", self.lru), (3, "s5", self.s5)):
            if b in self.mixers:
                P.cur = name
                self.obr = Tile(self.obr_all.t[:, b * NFT:(b + 1) * NFT, :], f"obr{b}", 0)
                self.obr.res = self.obr_all.res[b * NFT:(b + 1) * NFT]
                self.obr.parts = True
                fn(l)
                if b == 0:
                    self.dbg("obr", self.obr.all())
        if self.stage < 7:
            return
        self.mpool = [[5, 6, 7], 0]
        for b in range(4):
            if b in self.mixers:
                P.cur = f"merge{b}"
                self.obr = Tile(self.obr_all.t[:, b * NFT:(b + 1) * NFT, :], f"obr{b}", 0)
                self.obr.res = self.obr_all.res[b * NFT:(b + 1) * NFT]
                self.obr.parts = True
                self.merge(l)
        if self.stage < 8:
            return
        P.cur = "outproj"
        self.outproj(l)

    def merge(self, l):
        P = self.P
        for dm in range(KC):
            if dm % 4 == 0:
                if dm:
                    self.wdone(2)
                m, r = self.wnext("mg")
                wb, rb = self.wnext("br")
            gps = self.nb(self.mpool)
            bps = self.nb(self.mpool)
            c0 = (dm % 4) * 128
            self.proj_fm(gps.all(), m, r, c0, 128, self.u)
            self.proj_fm(bps.all(), wb, rb, c0, 128, self.obr, nk=4)
            g = self.mgt[dm % 2].all()
            P.act(g, gps.all(), AF.Sigmoid)
            if self.first_branch:
                P.tt("dve", self.mg[:, dm, :], g, bps.all(), ALU.mult)
            else:
                tmp = self.mgt[2].all()
                P.tt("dve", tmp, g, bps.all(), ALU.mult)
                P.tt("pool", self.mg[:, dm, :], self.mg[:, dm, :], tmp, ALU.add)
        self.wdone(2)
        self.first_branch = False

    def outproj(self, l):
        P = self.P
        mb = self.u
        for k in range(KC):
            P.copy("act", mb[:, k, :], self.mg[:, k, :])
        for dm in range(KC):
            if dm % 4 == 0:
                if dm:
                    self.wdone(1)
                w, r = self.wnext("out")
            ps = self.nb()
            self.proj_fm(ps.all(), w, r, (dm % 4) * 128, 128, mb)
            P.tt("dve", self.h[:, dm, :], self.h[:, dm, :], ps.all(), ALU.add)
        self.wdone(1)

    def la_core(self, segs, qd, kd, kt, decay, st, norm):
        P = self.P
        tb_bank = self.nb()
        tbv = tb_bank.t[:].bitcast(BF16)
        for tb in range(NTB):
            for tile in range(2):
                c0 = (tb * 2 + tile) * 128
                P.tr(V(tbv[:, c0:c0 + 128], tb_bank.res), kt[tile][:, tb * 128:(tb + 1) * 128], self.idn.all())
        P.copy("act", V(self.kttok.t[:].rearrange("p a n -> p (a n)"), self.kttok.res), V(tbv, tb_bank.res))
        if self.stage < 5.1:
            return
        kvb = [[self.nb(), self.nb()] for _ in range(2)]
        for tile in range(2):
            for h in range(4):
                for (tl, r0, K) in segs[h]:
                    if tl != tile:
                        continue
                    for c in range(NCH):
                        tb, half = c // 2, c % 2
                        bank = kvb[tile][half]
                        cc = tb * 128
                        P.mm(V(bank.t[r0:r0 + K, cc:cc + 128], bank.res),
                             V(self.kttok.t[half * 64:(half + 1) * 64, tb, tile * 128 + r0:tile * 128 + r0 + K], (self.kttok.res[tb],)),
                             V(self.vtok.t[half * 64:(half + 1) * 64, tb, h * 128:(h + 1) * 128], (self.vtok.res[tb],)),
                             start=True, stop=True)
        if self.stage < 5.2:
            return
        for tile in range(2):
            P.copy("pool", self.S32[:, tile, 0, :], st[:, tile, :])
            for c in range(NCH):
                bank = kvb[tile][c % 2]
                cc = (c // 2) * 128
                P.stt(self.S32[:, tile, c + 1, :], self.S32[:, tile, c, :], decay(tile, c),
                      V(bank.t[:, cc:cc + 128], bank.res), ALU.mult, ALU.add)
            P.copy("pool", st[:, tile, :], self.S32[:, tile, NCH, :])
            P.copy("act", self.Sbf[:, tile, :, :], V(self.S32.t[:, tile, 0:NCH, :], (self.S32.res[tile],)))
        if self.stage < 5.3:
            return
        for h in range(4):
            sps = self.nb()
            n = len(segs[h])
            for tb in range(NTB):
                for si, (tl, r0, K) in enumerate(segs[h]):
                    P.mm(V(sps.t[:, tb * 128:(tb + 1) * 128], sps.res),
                         V(kd[tl].ap[r0:r0 + K, tb * 128:(tb + 1) * 128], kd[tl].res),
                         V(qd[tl].ap[r0:r0 + K, tb * 128:(tb + 1) * 128], qd[tl].res),
                         start=(si == 0), stop=(si == n - 1))
            for tb in range(NTB):
                P.tt("dve", V(self.sT.t[:, tb, h, :], (self.sT.res[tb],)), V(sps.t[:, tb * 128:(tb + 1) * 128], sps.res),
                     self.cst[:, 0:128], ALU.mult)
        if self.stage < 5.4:
            return
        for h in range(4):
            ops = self.nb()
            for tb in range(NTB):
                P.mm(V(ops.t[:, tb * 128:(tb + 1) * 128], ops.res),
                     V(self.vtok.t[:, tb, h * 128:(h + 1) * 128], (self.vtok.res[tb],)),
                     V(self.sT.t[:, tb, h, :], (self.sT.res[tb],)), start=True, stop=False)
                for half in range(2):
                    c = tb * 2 + half
                    n = len(segs[h])
                    for si, (tl, r0, K) in enumerate(segs[h]):
                        P.mm(V(ops.t[:, c * 64:(c + 1) * 64], ops.res),
                             V(self.Sbf.t[r0:r0 + K, tl, c, :], (self.Sbf.res[tl],)),
                             V(qd[tl].ap[r0:r0 + K, c * 64:(c + 1) * 64], qd[tl].res),
                             start=False, stop=(half == 1 and si == n - 1))
            if self.stage >= 5.5:
                norm(h, ops)

    def gates(self, wg, rg, g0):
        P = self.P
        for ft in range(NFT):
            ps = self.nb()
            self.proj_fm(ps.all(), wg, rg, g0 + ft * 128, 128, self.u)
            P.act(self.sg[:, ft, :], ps.all(), AF.Silu)

    def vproj(self, wv, rv):
        P = self.P
        for tb in range(NTB):
            ps = self.nb()
            for k in range(KC):
                P.mm(ps.all(), self.u[:, k, tb * 128:(tb + 1) * 128], V(wv.t[:, k, 0:512], rv),
                     start=(k == 0), stop=(k == KC - 1))
            P.copy("act", self.vtok[:, tb, :], ps.all())

    def gla(self, l):
        P = self.P
        f, b = self.f, self.b
        self.mx_switch("la")
        w1, r1 = self.wnext("in")
        lrp = self.nb()
        self.proj_fm(V(lrp.t[0:16, :], lrp.res), w1, r1, 0, 16, self.u)
        self.gates(w1, r1, 16)
        self.wdone(1)
        w2, r2 = self.wnext("in")
        lrb = V(b[0].t[0:16, :], b[0].res)
        P.copy("act", lrb, V(lrp.t[0:16, :], lrp.res))
        eq = [f[0].all(), f[1].all()]
        ek = [f[2].all(), f[3].all()]
        for tile in range(2):
            zp = self.nb()
            P.mm(zp.all(), V(self.wlr.t[0:16, l, tile * 128:(tile + 1) * 128], self.wlr.res), lrb, start=True, stop=True)
            e1 = f[4].all()
            P.act(e1, zp.all(), AF.Exp, bias=self.nblr[:, l, tile:tile + 1], scale=-1.0)
            P.act(e1, e1, AF.Ln, bias=self.oneb[:, 0:1])
            cum = f[5].all()
            P.scan(cum, self.cst[:, 128:640], e1, 0.0)
            P.act(eq[tile], cum, AF.Exp, scale=-1.0 / 16.0)
            P.act(ek[tile], cum, AF.Exp, scale=1.0 / 16.0)
        qd = [b[1].all(), b[2].all()]
        kd = [b[3].all(), b[4].all()]
        kt = [b[5].all(), b[6].all()]
        for tile in range(2):
            qp = self.nb()
            self.proj_fm(qp.all(), w2, r2, tile * 128, 128, self.u)
            P.stt(qd[tile], qp.all(), 0.125, eq[tile], ALU.mult, ALU.mult)
            kp = self.nb()
            self.proj_fm(kp.all(), w2, r2, 256 + tile * 128, 128, self.u)
            kd32 = f[6 + tile].all()
            P.tt("dve", kd32, kp.all(), ek[tile], ALU.mult)
            P.copy("act", kd[tile], kd32)
            eql = V(eq[tile].ap.rearrange("p (c t) -> p c t", t=64)[:, :, 63:64].to_broadcast([128, NCH, 64]), eq[tile].res)
            P.tt("dve", V(kt[tile].ap.rearrange("p (c t) -> p c t", t=64), kt[tile].res),
                 V(kd32.ap.rearrange("p (c t) -> p c t", t=64), kd32.res), eql, ALU.mult)
        self.wdone(1)
        w3, r3 = self.wnext("in")
        self.vproj(w3, r3)
        self.wdone(1)
        self.dbg("sg", self.sg.all())
        self.dbg("qd0", qd[0])
        self.dbg("kd0", kd[0])
        self.dbg("kt0", kt[0])
        self.dbg("eq0", eq[0])
        self.dbg("vtok", self.vtok.all())

        if self.stage < 5:
            return

        def decay(tile, c):
            return eq[tile][:, c * 64 + 63:c * 64 + 64]

        def norm(h, ops):
            osq = b[7].all()
            P.act(osq, ops.all(), AF.Square)
            o32 = f[8].all()
            P.copy("act", o32, ops.all())
            sp = self.nb()
            P.mm(sp.all(), self.ones.all(), osq, start=True, stop=True)
            rs = f[9].all()
            P.act(rs, sp.all(), AF.Ln, bias=self.epsb[:, 0:1], scale=1.0 / 128.0)
            P.act(rs, rs, AF.Exp, scale=-0.5)
            P.stt(o32, o32, self.pp[:, l, 10 + h:11 + h], rs, ALU.mult, ALU.mult)
            P.tt("dve", self.obr[:, h, :], o32, self.sg[:, h, :], ALU.mult)

        self.la_core(GLA_SEGS, qd, kd, kt, decay, V(self.st_gla.t[:, l], (self.st_gla.res[l],)), norm)

    def ret(self, l):
        P = self.P
        f, b = self.f, self.b
        self.mx_switch("la")
        w1, r1 = self.wnext("in")
        ws, rs_ = self.wnext("in")
        qd = [b[1].all(), b[2].all()]
        kd = [b[3].all(), b[4].all()]
        kt = [b[5].all(), b[6].all()]
        kd32 = [f[6].all(), f[7].all()]
        for a in range(2):
            for tile in range(2):
                pn = self.nb()
                self.proj_fm(pn.all(), w1, r1, a * 256 + tile * 128, 128, self.u)
                px = self.nb()
                self.proj_fm(px.all(), ws, rs_, a * 256 + tile * 128, 128, self.u)
                cs, sn = self.rt[:, tile, 2 * a, :], self.rt[:, tile, 2 * a + 1, :]
                m1, m2 = f[0 + 2 * tile].all(), f[1 + 2 * tile].all()
                P.tt("dve", m1, pn.all(), cs, ALU.mult)
                P.tt("dve", m2, px.all(), sn, ALU.mult)
                if a == 0:
                    P.tt("pool", qd[tile], m1, m2, ALU.add)
                else:
                    P.tt("pool", kd32[tile], m1, m2, ALU.add)
                    P.copy("act", kd[tile], kd32[tile])
                    P.act(kt[tile], kd32[tile], AF.Copy, scale=self.g64[:, tile:tile + 1])
        self.wdone(1)
        self.wdone(1)
        w2, r2 = self.wnext("in")
        self.vproj(w2, r2)
        self.wdone(1)
        w3, r3 = self.wnext("in")
        self.gates(w3, r3, 0)
        self.wdone(1)

        def decay(tile, c):
            return self.g64[:, tile:tile + 1]

        def norm(h, ops):
            ob, osq = b[7].all(), b[0].all()
            P.copy("act", ob, ops.all())
            P.act(osq, ops.all(), AF.Square)
            o32 = f[8].all()
            P.copy("act", o32, ops.all())
            s1 = self.nb()
            s2 = self.nb()
            P.mm(s1.all(), self.ones.all(), ob, start=True, stop=True)
            P.mm(s2.all(), self.ones.all(), osq, start=True, stop=True)
            mean, var = f[4].all(), f[5].all()
            P.act(mean, s1.all(), AF.Copy, scale=1.0 / 128.0)
            P.tt("dve", var, mean, mean, ALU.mult)
            P.stt(var, s2.all(), 1.0 / 128.0, var, ALU.mult, ALU.subtract)
            P.act(var, var, AF.Ln, bias=self.epsb[:, 0:1])
            P.act(var, var, AF.Exp, scale=-0.5)
            P.tt("dve", o32, o32, mean, ALU.subtract)
            P.stt(o32, o32, self.pp[:, l, 14 + h:15 + h], var, ALU.mult, ALU.mult)
            P.stt(self.obr[:, h, :], o32, self.pp[:, l, 18 + h:19 + h], self.sg[:, h, :], ALU.add, ALU.mult)

        self.la_core(GLA_SEGS, qd, kd, kt, decay, V(self.st_ret.t[:, l], (self.st_ret.res[l],)), norm)

    def lru(self, l):
        P = self.P
        f, b = self.f, self.b
        self.mx_switch("lru")
        w1, r1 = self.wnext("in")
        P.dma("sp", V(self.lruw.t[:].rearrange("p a f n -> p (a f n)"), self.lruw.res), V(self.sc["lruw"][l], (self.scres["lruw"][l],)))
        stl = (self.st_lru.res[l],)
        sth = (self.st_lxh.res[l],)
        for ft in range(NFT):
            ps = self.nb()
            self.proj_fm(ps.all(), w1, r1, ft * 128, 128, self.u)
            P.copy("pool", V(self.lxh.t[:, ft, 0:3], (self.lxh.res[ft],)), V(self.st_lxh.t[:, l, ft, 0:3], sth))
            P.copy("act", V(self.lxh.t[:, ft, 3:3 + T], (self.lxh.res[ft],)), ps.all())
            P.copy("pool", V(self.st_lxh.t[:, l, ft, 0:3], sth), V(self.lxh.t[:, ft, T:T + 3], (self.lxh.res[ft],)))
        self.wdone(1)
        w2, r2 = self.wnext("in")
        self.gates(w2, r2, 0)
        self.wdone(1)
        for ft in range(NFT):
            xc = f[0 + (ft % 2) * 5].all()
            cw = lambda w: self.pp[:, l, 22 + ft * 4 + w:23 + ft * 4 + w]
            P.ts("dve", xc, V(self.lxh.t[:, ft, 0:T], (self.lxh.res[ft],)), cw(0), self.pp[:, l, 38 + ft:39 + ft], ALU.mult, ALU.add)
            for w in range(1, 4):
                P.stt(xc, V(self.lxh.t[:, ft, w:w + T], (self.lxh.res[ft],)), cw(w), xc, ALU.mult, ALU.add)
            xcb = b[ft % 2].all()
            P.copy("act", xcb, xc)
            rp = self.nb()
            ip = self.nb()
            P.mm(rp.all(), V(self.lruw.t[:, 0, ft, :], self.lruw.res), xcb, start=True, stop=True)
            P.mm(ip.all(), V(self.lruw.t[:, 1, ft, :], self.lruw.res), xcb, start=True, stop=True)
            a, mu, ig = f[1 + (ft % 2) * 5].all(), f[2 + (ft % 2) * 5].all(), f[3 + (ft % 2) * 5].all()
            P.act(a, rp.all(), AF.Sigmoid, bias=self.pp[:, l, 42 + ft:43 + ft])
            P.act(a, a, AF.Exp, scale=self.lsc[:, l, ft:ft + 1])
            P.act(mu, a, AF.Square)
            P.act(mu, mu, AF.Sqrt, bias=self.oneb[:, 0:1], scale=-1.0)
            P.act(ig, ip.all(), AF.Sigmoid, bias=self.pp[:, l, 46 + ft:47 + ft])
            P.tt("dve", ig, ig, xc, ALU.mult)
            P.tt("dve", ig, ig, mu, ALU.mult)
            hs = f[4 + (ft % 2) * 5].all()
            P.scan(hs, a, ig, V(self.st_lru.t[:, l, ft:ft + 1], stl))
            P.copy("pool", V(self.st_lru.t[:, l, ft:ft + 1], stl), hs[:, T - 1:T])
            P.tt("pool", self.obr[:, ft, :], hs, self.sg[:, ft, :], ALU.mult)

    def s5(self, l):
        P = self.P
        f, b = self.f, self.b
        self.mx_switch("s5")
        w1, r1 = self.wnext("in")
        P.dma("sp", V(self.bbw.t[:].rearrange("p r a n -> p r (a n)"), self.bbw.res), V(self.bbscr[l], (self.bbres[l],)))
        P.dma("sp", V(self.cTw.t[:].rearrange("p a g n -> p (a g n)"), self.cTw.res), V(self.sc["s5cT"][l], (self.scres["s5cT"][l],)))
        P.dma("sp", V(self.dD.t[:].rearrange("p a n -> p (a n)"), self.dD.res), V(self.ddscr[l], (self.ddres[l],)))
        P.act(self.cTw[:, 1], self.cTw[:, 1], AF.Copy, scale=-1.0)
        sts = (self.st_s5.res[l],)
        sub = [b[k].all() for k in range(4)]
        for ft in range(NFT):
            ps = self.nb()
            self.proj_fm(ps.all(), w1, r1, ft * 128, 128, self.u)
            P.copy("act", sub[ft], ps.all())
        self.wdone(1)
        w2, r2 = self.wnext("in")
        self.gates(w2, r2, 0)
        self.wdone(1)
        y5b = [b[4 + k].all() for k in range(4)]
        yps = None
        bupool = [[0, 1, 2, 3], 0]
        ypool = [[4], 0]
        for gp in range(16):
            ft, q = gp // 4, gp % 4
            q2, e = q // 2, q % 2
            par = gp % 2
            rb = self.rot[par]
            P.dma("sp", rb.all(), V(self.rotscr[l, gp], (self.rotres[l][gp],)))
            C, Sn = rb[:, 0, :], rb[:, 1, :]
            brp, bip = self.nb(bupool), self.nb(bupool)
            rows = slice(64 * q2, 64 * q2 + 64)
            P.mm(brp.all(), V(self.bbw.t[rows, 0, ft * 2 + e, :], self.bbw.res), V(sub[ft].ap[rows, :], sub[ft].res), start=True, stop=True)
            P.mm(bip.all(), V(self.bbw.t[rows, 1, ft * 2 + e, :], self.bbw.res), V(sub[ft].ap[rows, :], sub[ft].res), start=True, stop=True)
            t1, t2, t3, t4 = (f[4 * par + k].all() for k in range(4))
            P.tt("dve", t1, brp.all(), C, ALU.mult)
            P.tt("dve", t2, bip.all(), Sn, ALU.mult)
            P.tt("pool", t1, t1, t2, ALU.add)
            P.tt("dve", t3, bip.all(), C, ALU.mult)
            P.tt("dve", t4, brp.all(), Sn, ALU.mult)
            P.tt("pool", t3, t3, t4, ALU.subtract)
            rho_bc = V(self.rho.t[:, l, gp:gp + 1].to_broadcast([128, T]), self.rho.res)
            P.scan(t2, rho_bc, t1, V(self.st_s5.t[:, l, 0, gp:gp + 1], sts))
            P.scan(t4, rho_bc, t3, V(self.st_s5.t[:, l, 1, gp:gp + 1], sts))
            tl = slice(T - 1, T)
            P.tt("dve", self.Hs[:, par, 0, :], t2, C, ALU.mult)
            P.stt(self.Hs[:, par, 1, :], t4, -1.0, Sn, ALU.mult, ALU.mult)
            P.tt("dve", self.Hs[:, par, 2, :], t4, C, ALU.mult)
            P.tt("pool", self.Hs[:, par, 3, :], t2, Sn, ALU.mult)
            ca, cb = t1[:, 0:1], t1[:, 1:2]
            P.tt("dve", ca, t4[:, tl], Sn[:, tl], ALU.mult)
            P.tt("dve", cb, t2[:, tl], Sn[:, tl], ALU.mult)
            P.stt(V(self.st_s5.t[:, l, 0, gp:gp + 1], sts), t2[:, tl], C[:, tl], ca, ALU.mult, ALU.subtract)
            P.stt(V(self.st_s5.t[:, l, 1, gp:gp + 1], sts), t4[:, tl], C[:, tl], cb, ALU.mult, ALU.add)
            if q == 0:
                yps = self.nb(ypool)
            if q == 0:
                P.mm(yps.all(), self.dD[:, ft, :], sub[ft], start=True, stop=False)
            for k, wsel in enumerate((0, 0, 1, 1)):
                P.mm(yps.all(), V(self.cTw.t[:, wsel, gp, :], self.cTw.res), self.Hs[:, par, k, :], start=False, stop=(q == 3 and k == 3))
            if q == 3:
                y1, y2, inner, sgm = f[8].all(), f[9].all(), f[8].all(), f[9].all()
                yf = self.y5[:, ft, :]
                P.act(y2, yps.all(), AF.Square)
                P.ts("dve", y2, y2, 0.044715, 1.0, ALU.mult, ALU.add)
                P.tt("dve", y1, y2, yps.all(), ALU.mult)
                P.act(sgm, y1, AF.Sigmoid, scale=2.0 * GELU_C)
                P.tt("dve", yf, sgm, yps.all(), ALU.mult)
                P.copy("act", y5b[ft], yf)
        w3, r3 = self.wnext("glu")
        for fo in range(NFT):
            ps = self.nb(bupool)
            for k in range(4):
                P.mm(ps.all(), V(w3.t[:, k, fo * 128:(fo + 1) * 128], r3), y5b[k], start=(k == 0), stop=(k == 3))
            sig = f[8 + fo % 2].all()
            P.act(sig, ps.all(), AF.Sigmoid, bias=self.pp[:, l, 58 + fo:59 + fo])
            P.tt("dve", sig, sig, self.y5[:, fo, :], ALU.mult)
            P.tt("pool", self.obr[:, fo, :], sig, self.sg[:, fo, :], ALU.mult)
        self.wdone(1)


_CACHE = {}


def _get_nc(key, **kw):
    if key not in _CACHE:
        _CACHE[key] = Builder(**kw).build()
    return _CACHE[key]


def _weights_maps(inputs, layers):
    pk = _pack_params(inputs, layers)
    c, idn, tpos = _consts()
    f32 = lambda a: np.ascontiguousarray(np.asarray(a, dtype=np.float32))
    m = dict(pk)
    m["w_in"] = f32(np.asarray(inputs["w_in"])[layers])
    m["w_mg"] = f32(np.asarray(inputs["w_merge_gate"])[layers])
    m["w_br"] = f32(np.asarray(inputs["w_branch"])[layers])
    m["w_out"] = f32(np.asarray(inputs["w_out"])[layers])
    m["w_glu"] = f32(np.asarray(inputs["s5_glu_w"])[layers])
    m["fg"] = f32(np.asarray(inputs["final_norm_gain"]).reshape(8, 128).T)
    m["cst"] = c
    m["idn"] = idn
    m["tpos"] = tpos
    return m


def kernel(**inputs):
    x = np.asarray(inputs["x"], dtype=np.float32)
    B, S, _ = x.shape
    n_cores = 8
    n_seq = B // n_cores
    layers = list(range(DEPTH))
    nc = _get_nc(("full", n_seq, S), n_layers=DEPTH, n_seq=n_seq, tiles_per_seq=S // T, final_norm=True)
    wm = _weights_maps(inputs, layers)
    in_maps = []
    for c in range(n_cores):
        m = dict(wm)
        m["xT"] = np.ascontiguousarray(x[c * n_seq:(c + 1) * n_seq].transpose(0, 2, 1))
        in_maps.append(m)
    res = run_bass_kernel_spmd(nc, in_maps, core_ids=list(range(n_cores)))
    out = np.empty((B, S, D), np.float32)
    for c in range(n_cores):
        out[c * n_seq:(c + 1) * n_seq] = res.results[c]["yT"].transpose(0, 2, 1)
    return out
```

```python
import math
from contextlib import ExitStack

import numpy as np
import concourse.bass as bass
import concourse.mybir as mybir
from concourse.bass_utils import run_bass_kernel_spmd

F32 = mybir.dt.float32
BF16 = mybir.dt.bfloat16
I32 = mybir.dt.int32
AF = mybir.ActivationFunctionType
ALU = mybir.AluOpType

D = 1024
KC = 8
T = 512
NTB = T // 128
NCH = T // 64
W = 512
NFT = 4
DEPTH = 4
IN_COLS = 5136
EPS = 1e-6
C_GQ, C_GK, C_GV, C_GLR, C_GG = 0, 256, 512, 1024, 1040
C_RQ, C_RK, C_RV, C_RG = 1552, 1808, 2064, 2576
C_LX, C_LG, C_SU, C_SG = 3088, 3600, 4112, 4624
NPP = 110
TWO_PI = 2.0 * math.pi
CW1 = 6.28125
CW2 = TWO_PI - CW1
GELU_C = math.sqrt(2.0 / math.pi)


class Res:
    __slots__ = ("name", "lw", "rd")

    def __init__(self, name):
        self.name = name
        self.lw = None
        self.rd = set()


class V:
    __slots__ = ("ap", "res")

    def __init__(self, ap, res):
        self.ap = ap
        self.res = tuple(res)

    def __getitem__(self, key):
        return V(self.ap[key], self.res)


class Tile:
    def __init__(self, t, name, nparts):
        self.t = t
        self.name = name
        self.res = tuple(Res(f"{name}.{i}") for i in range(max(1, nparts)))
        self.parts = nparts > 0

    def __getitem__(self, key):
        if self.parts and isinstance(key, tuple) and len(key) >= 2 and isinstance(key[1], int):
            return V(self.t[key], (self.res[key[1]],))
        return V(self.t[key], self.res)

    def all(self):
        return V(self.t[:], self.res)


ENGS = ("pe", "act", "dve", "pool", "sp")


class Node:
    __slots__ = ("id", "q", "fn", "dma", "deps", "odeps", "dur", "nbytes", "is_out", "tag", "t0", "t1")

    def __init__(self, id, q, fn, dma, deps, odeps, dur, nbytes=0):
        self.id, self.q, self.fn, self.dma = id, q, fn, dma
        self.deps, self.odeps, self.dur, self.nbytes = deps, odeps, dur, nbytes
        self.is_out = False


class Prog:
    NDS = 16
    NSW = 76
    DMA_BW = 150.0

    def __init__(self, nc, stack, reorder=True):
        self.nc = nc
        self.stack = stack
        self.nodes = []
        self.cur = ""
        self.why = None
        self.reorder = reorder
        self.esem = {e: stack.enter_context(nc.semaphore(f"s_{e}")) for e in ENGS}
        self.dsem = [stack.enter_context(nc.semaphore(f"d_{i}")) for i in range(self.NDS)]

    def sb(self, name, shape, dtype, nparts=0):
        t = self.stack.enter_context(self.nc.sbuf_tensor("sb_" + name, list(shape), dtype))
        return Tile(t, name, nparts)

    def ps(self, name, shape, dtype=F32):
        t = self.stack.enter_context(self.nc.psum_tensor("ps_" + name, list(shape), dtype))
        return Tile(t, name, 0)

    def _collect(self, q, reads, writes):
        deps, odeps = set(), set()
        nodes = self.nodes
        why = self.why
        for r in reads:
            if r.lw is not None:
                deps.add(r.lw)
                if why is not None:
                    why[(len(nodes), r.lw)] = ("RAW", r.name)
        for w in writes:
            if w.lw is not None:
                if q == "pe" and nodes[w.lw].q == "pe" and not nodes[w.lw].dma:
                    odeps.add(w.lw)
                else:
                    deps.add(w.lw)
                if why is not None:
                    why[(len(nodes), w.lw)] = ("WAW", w.name)
            deps.update(w.rd)
            if why is not None:
                for x in w.rd:
                    why[(len(nodes), x)] = ("WAR", w.name)
        return deps, odeps

    def _add(self, q, fn, dma, reads, writes, dur, nbytes=0):
        deps, odeps = self._collect(q, reads, writes)
        n = Node(len(self.nodes), q, fn, dma, deps, odeps - deps, dur, nbytes)
        n.tag = self.cur
        self.nodes.append(n)
        for r in reads:
            r.rd.add(n.id)
        for w in writes:
            w.lw = n.id
            w.rd = set()
        return n

    def op(self, eng, fn, reads, writes, dur=300.0):
        return self._add(eng, fn, False, reads, writes, dur)

    def handoff(self, old, new):
        toks = set()
        for r in old:
            if r.lw is not None:
                toks.add(r.lw)
            toks.update(r.rd)
        for r in new:
            r.lw = None
            r.rd = set(toks)

    def dma(self, q, out, in_, is_output=False, **kw):
        oap, iap = out.ap, in_.ap
        nbytes = 1
        for d in oap.shape:
            nbytes *= int(d)
        nbytes *= 4 if oap.dtype == F32 else 2
        n = self._add(q, lambda e: e.dma_start(out=oap, in_=iap, **kw), True, in_.res, out.res, 0.0, nbytes)
        n.is_out = is_output
        return n

    def _schedule(self):
        import heapq
        nodes = self.nodes
        N = len(nodes)
        order = {e: [] for e in ENGS}
        if not self.reorder:
            for n in nodes:
                order[n.q].append(n.id)
            return order
        succ = [[] for _ in range(N)]
        cnt = [0] * N
        for n in nodes:
            ds = n.deps | n.odeps
            cnt[n.id] = len(ds)
            for d in ds:
                succ[d].append(n.id)
        ready = {e: [] for e in ENGS}
        for n in nodes:
            if cnt[n.id] == 0:
                heapq.heappush(ready[n.q], n.id)
        free_at = {e: 0.0 for e in ENGS}
        busy = {e: False for e in ENGS}
        events = []
        dma_free = 0.0
        now = 0.0
        done = 0

        def dispatch(e, now):
            nonlocal dma_free
            if busy[e] or not ready[e]:
                return
            i = heapq.heappop(ready[e])
            n = nodes[i]
            order[e].append(i)
            busy[e] = True
            n.t0 = now
            if n.dma:
                issue = 1500.0 if e == "pool" else 100.0
                t0 = max(now + issue, dma_free)
                dma_free = t0 + n.nbytes / self.DMA_BW
                n.t1 = dma_free + 2000.0
                heapq.heappush(events, (now + issue, 0, e_idx[e]))
                heapq.heappush(events, (dma_free + 2000.0, 1, i))
            else:
                n.t1 = now + n.dur + 60.0
                heapq.heappush(events, (now + n.dur, 0, e_idx[e]))
                heapq.heappush(events, (now + n.dur + 60.0, 1, i))

        e_idx = {e: k for k, e in enumerate(ENGS)}
        for e in ENGS:
            dispatch(e, 0.0)
        while events:
            now, kind, x = heapq.heappop(events)
            if kind == 0:
                e = ENGS[x]
                busy[e] = False
                dispatch(e, now)
            else:
                done += 1
                for s_ in succ[x]:
                    cnt[s_] -= 1
                    if cnt[s_] == 0:
                        q = nodes[s_].q
                        heapq.heappush(ready[q], s_)
                        dispatch(q, now)
        assert done == N, (done, N)
        self.sim_ns = now
        return order

    def emit(self):
        nc = self.nc
        nodes = self.nodes
        order = self._schedule()
        tok = [None] * len(nodes)
        sems = {("e", e): self.esem[e] for e in ENGS}
        for i in range(self.NDS):
            sems[("d", i)] = self.dsem[i]
        extra = {}
        nsw = 0
        dcnt = [0] * self.NDS
        dnext = 0
        for e in ENGS:
            pos = 0
            for i in order[e]:
                n = nodes[i]
                if not n.dma:
                    pos += 1
                    tok[i] = (("e", e), pos)
                elif e == "pool":
                    assert nsw < self.NSW, "out of one-shot semaphores"
                    key = ("w", nsw)
                    sems[key] = self.stack.enter_context(nc.semaphore(f"w_{nsw}"))
                    nsw += 1
                    tok[i] = (key, 16)
                else:
                    k = dnext
                    dnext = (dnext + 1) % self.NDS
                    if dcnt[k]:
                        extra[i] = (("d", k), dcnt[k])
                    dcnt[k] += 16
                    tok[i] = (("d", k), dcnt[k])
        fin = {}
        for i, n in enumerate(nodes):
            k, v = tok[i]
            if fin.get(k, 0) < v:
                fin[k] = v
        block = self.stack.enter_context(nc.Block())

        def replay(eng_handle, e):
            seen = {}
            for i in order[e]:
                n = nodes[i]
                need = {}
                for d in n.deps:
                    k, v = tok[d]
                    if need.get(k, 0) < v:
                        need[k] = v
                if i in extra:
                    k, v = extra[i]
                    if need.get(k, 0) < v:
                        need[k] = v
                for k, v in need.items():
                    if seen.get(k, 0) < v:
                        seen[k] = v
                        eng_handle.wait_ge(sems[k], v)
                k, v = tok[i]
                n.fn(eng_handle).then_inc(sems[k], 16 if n.dma else 1)
            if e == "sp":
                for k, v in fin.items():
                    if seen.get(k, 0) < v:
                        eng_handle.wait_ge(sems[k], v)

        @block.tensor
        def _(e):
            replay(e, "pe")

        @block.scalar
        def _(e):
            replay(e, "act")

        @block.vector
        def _(e):
            replay(e, "dve")

        @block.gpsimd
        def _(e):
            replay(e, "pool")

        @block.sync
        def _(e):
            replay(e, "sp")

    def finish(self):
        pass

    @staticmethod
    def _sc(x, reads):
        if isinstance(x, V):
            reads.extend(x.res)
            return x.ap
        return x

    @staticmethod
    def _n(v):
        n = 1
        for d in v.ap.shape[1:]:
            n *= int(d)
        return n

    def act(self, out, in_, func, bias=0.0, scale=1.0):
        reads = list(in_.res)
        b = self._sc(bias, reads)
        s = self._sc(scale, reads)
        o, i = out.ap, in_.ap
        return self.op("act", lambda e: e.activation(out=o, in_=i, func=func, bias=b, scale=s), reads, out.res,
                       dur=220.0 + 0.85 * self._n(in_))

    def tt(self, eng, out, in0, in1, op):
        o, a, b = out.ap, in0.ap, in1.ap
        n = self._n(in0)
        dur = (100.0 + 1.05 * n) if eng == "dve" else (150.0 + 2.6 * n)
        return self.op(eng, lambda e: e.tensor_tensor(out=o, in0=a, in1=b, op=op), in0.res + in1.res, out.res, dur=dur)

    def ts(self, eng, out, in0, s1, s2, op0, op1=None):
        reads = list(in0.res)
        a1 = self._sc(s1, reads)
        a2 = self._sc(s2, reads)
        o, a = out.ap, in0.ap
        n = self._n(in0)
        dur = (100.0 + 1.05 * n) if eng == "dve" else (150.0 + 2.6 * n)
        if op1 is None:
            return self.op(eng, lambda e: e.tensor_scalar(out=o, in0=a, scalar1=a1, scalar2=None, op0=op0), reads, out.res, dur=dur)
        return self.op(eng, lambda e: e.tensor_scalar(out=o, in0=a, scalar1=a1, scalar2=a2, op0=op0, op1=op1), reads, out.res, dur=dur)

    def stt(self, out, in0, scalar, in1, op0, op1):
        reads = list(in0.res) + list(in1.res)
        s = self._sc(scalar, reads)
        o, a, b = out.ap, in0.ap, in1.ap
        return self.op("dve", lambda e: e.scalar_tensor_tensor(out=o, in0=a, scalar=s, in1=b, op0=op0, op1=op1), reads, out.res,
                       dur=100.0 + 1.05 * self._n(in0))

    def scan(self, out, d0, d1, init):
        reads = list(d0.res) + list(d1.res)
        ini = self._sc(init, reads)
        o, a, b = out.ap, d0.ap, d1.ap
        return self.op("dve", lambda e: e.tensor_tensor_scan(out=o, data0=a, data1=b, initial=ini, op0=ALU.mult, op1=ALU.add), reads, out.res,
                       dur=100.0 + 2.1 * self._n(d1))

    def copy(self, eng, out, in_):
        o, i = out.ap, in_.ap
        n = self._n(in_)
        if eng == "act":
            return self.op("act", lambda e: e.activation(out=o, in_=i, func=AF.Copy), in_.res, out.res, dur=220.0 + 0.85 * n)
        dur = (100.0 + 1.05 * n) if eng == "dve" else (150.0 + 2.6 * n)
        return self.op(eng, lambda e: e.tensor_copy(out=o, in_=i), in_.res, out.res, dur=dur)

    def recip(self, out, in_):
        o, i = out.ap, in_.ap
        return self.op("dve", lambda e: e.reciprocal(out=o, in_=i), in_.res, out.res, dur=100.0 + 6.5 * self._n(in_))

    def memset(self, eng, out, val):
        o = out.ap
        return self.op(eng, lambda e: e.memset(o, val), (), out.res, dur=100.0 + 0.5 * self._n(out))

    def mm(self, out, lhsT, rhs, start, stop):
        o, l, r = out.ap, lhsT.ap, rhs.ap
        return self.op("pe", lambda e: e.matmul(o, l, r, start=start, stop=stop), lhsT.res + rhs.res, out.res,
                       dur=40.0 + 0.45 * max(64, self._n(rhs)))

    def tr(self, out, in_, ident):
        o, i, d = out.ap, in_.ap, ident.ap
        return self.op("pe", lambda e: e.transpose(o, i, d), in_.res + ident.res, out.res, dur=100.0)


def _pack_params(inp, layers):
    L = len(layers)
    pp = np.zeros((L, 128, NPP), np.float32)
    lruw = np.zeros((L, 128, 2, 4, 128), np.float32)
    cT = np.zeros((L, 128, 2, 16, 128), np.float32)
    bT = np.zeros((L, 2, 128, 4, 2, 128), np.float32)
    aT = np.zeros((L, 3, 128, 4, 2, 128), np.float32)
    wlr = np.zeros((L, 16, 256), np.float32)
    for li, l in enumerate(layers):
        g = lambda nm: np.asarray(inp[nm][l], np.float32)
        pp[li, :, 0:8] = g("norm_gain").reshape(8, 128).T
        pp[li, :, 8:10] = g("gla_b_lr").reshape(2, 128).T
        pp[li, :, 10:14] = g("gla_norm_gain").reshape(4, 128).T
        pp[li, :, 14:18] = g("ret_norm_gain").reshape(4, 128).T
        pp[li, :, 18:22] = g("ret_norm_bias").reshape(4, 128).T
        cw = g("lru_conv_w")
        pp[li, :, 22:38] = cw.reshape(4, 4, 128).transpose(2, 1, 0).reshape(128, 16)
        pp[li, :, 38:42] = g("lru_conv_b").reshape(4, 128).T
        pp[li, :, 42:46] = g("lru_b_a").reshape(4, 128).T
        pp[li, :, 46:50] = g("lru_b_x").reshape(4, 128).T
        pp[li, :, 50:54] = g("lru_lambda").reshape(4, 128).T
        pp[li, :, 54:58] = g("s5_d").reshape(4, 128).T
        pp[li, :, 58:62] = g("s5_glu_b").reshape(4, 128).T
        are = g("s5_a_re").reshape(16, 2, 64)
        aim = g("s5_a_im").reshape(16, 2, 64)
        ldt = g("s5_log_dt").reshape(16, 2)
        pp[li, :, 62:78] = are.transpose(1, 2, 0).reshape(128, 16)
        pp[li, :, 78:94] = aim.transpose(1, 2, 0).reshape(128, 16)
        pp[li, :, 94:110] = np.repeat(ldt.T[:, None, :], 64, axis=1).reshape(128, 16)
        ldt_b = np.repeat(ldt[:, :, None], 64, axis=2)
        for q2 in range(2):
            for ft in range(4):
                for e in range(2):
                    gp = 4 * ft + 2 * q2 + e
                    aT[li, 0, q2 * 64:(q2 + 1) * 64, ft, e, :] = are[gp].reshape(1, 128)
                    aT[li, 1, q2 * 64:(q2 + 1) * 64, ft, e, :] = aim[gp].reshape(1, 128)
                    aT[li, 2, q2 * 64:(q2 + 1) * 64, ft, e, :] = ldt_b[gp].reshape(1, 128)
        wlr[li] = g("gla_w_lr")
        for ax, nm in enumerate(("lru_w_a", "lru_w_x")):
            wv = g(nm)
            for blk in range(8):
                h2 = blk % 2
                lruw[li, h2 * 64:(h2 + 1) * 64, ax, blk // 2, h2 * 64:(h2 + 1) * 64] = wv[blk]
        for ri, nm in enumerate(("s5_c_re", "s5_c_im")):
            cv = g(nm).reshape(16, 2, 16, 64)
            for two in range(2):
                for gp in range(16):
                    c0 = (gp % 4) * 32 + two * 16
                    cT[li, two * 64:(two + 1) * 64, ri, gp, c0:c0 + 16] = cv[gp, two].T
        for ri, nm in enumerate(("s5_b_re", "s5_b_im")):
            bv = g(nm).reshape(16, 2, 64, 16)
            for two in range(2):
                for q2 in range(2):
                    for ft in range(4):
                        for e in range(2):
                            gp = 4 * ft + 2 * q2 + e
                            r0 = q2 * 64 + e * 32 + two * 16
                            bT[li, ri, r0:r0 + 16, ft, e, two * 64:(two + 1) * 64] = bv[gp, two].T
    return dict(pp=pp, lruw=lruw, s5cT=cT, s5bT=bT.reshape(L, 2, 128, 1024),
                s5aT=aT.reshape(L, 3, 128, 1024), wlr=wlr)


def _consts():
    c = np.zeros((128, 1216), np.float32)
    j = np.arange(128)[:, None]
    i = np.arange(128)[None, :]
    c[:, 0:128] = ((j // 64 == i // 64) & (i >= j)).astype(np.float32)
    t = np.arange(T)
    c[:, 128:640] = (t % 64 != 0).astype(np.float32)[None, :]
    c[:, 640:1152] = ((t % 64) + 1).astype(np.float32)[None, :]
    p = np.arange(128)
    ii = p % 32
    c[:, 1152] = (10000.0 ** (-(ii.astype(np.float64)) / 32.0)).astype(np.float32)
    for tile in range(2):
        hh = 2 * tile + p // 64
        c[:, 1153 + tile] = np.log1p(-np.exp2(-5.0 - hh.astype(np.float64))).astype(np.float32)
    c[:, 1155] = np.where((p % 64) < 32, -1.0, 1.0)
    idn = np.eye(128, dtype=np.float32)
    tpos = np.arange(T, dtype=np.float32)[None, :].repeat(128, 0)
    return c, idn, tpos


GLA_SEGS = [[(h // 2, (h % 2) * 64, 64)] for h in range(4)]
RET_SEGS = [[(0, 32 * h, 32), (1, 32 * h, 32)] for h in range(4)]


class Builder:
    def __init__(self, n_layers, n_seq, tiles_per_seq, final_norm=True, debug=None, mixers=(0, 1, 2, 3), stage=99):
        self.L = n_layers
        self.n_seq = n_seq
        self.tps = tiles_per_seq
        self.final_norm = final_norm
        self.debug = debug or []
        self.mixers = mixers
        self.stage = stage
        self.S = tiles_per_seq * T

    def build(self):
        L = self.L
        nc = bass.Bass("TRN2", target_bir_lowering=False)
        self.nc = nc
        S = self.S
        dr = {}

        def din(name, shape, dt=F32):
            dr[name] = nc.dram_tensor(name, list(shape), dt, kind="ExternalInput").ap()
            return dr[name]

        din("xT", [self.n_seq, D, S])
        din("w_in", [L, D, IN_COLS])
        din("w_mg", [L, 4, D, D])
        din("w_br", [L, 4, W, D])
        din("w_out", [L, D, D])
        din("w_glu", [L, W, W])
        din("pp", [L, 128, NPP])
        din("fg", [128, 8])
        din("lruw", [L, 128, 2, 4, 128])
        din("s5cT", [L, 128, 2, 16, 128])
        din("s5bT", [L, 2, 128, 1024])
        din("s5aT", [L, 3, 128, 1024])
        din("wlr", [L, 16, 256])
        din("cst", [128, 1216])
        din("idn", [128, 128])
        din("tpos", [128, T])
        self.yT = nc.dram_tensor("yT", [self.n_seq, D, S], F32, kind="ExternalOutput").ap()
        self.bbscr = nc.dram_tensor("bbscr", [L, 128, 2, 1024], BF16, kind="Internal").ap()
        self.rotscr = nc.dram_tensor("rotscr", [L, 16, 128, 2, T], F32, kind="Internal").ap()
        self.ddscr = nc.dram_tensor("ddscr", [L, 128, 512], BF16, kind="Internal").ap()
        self.ddres = [Res(f"ddscr{l}") for l in range(L)]
        def scr(name, shape):
            return nc.dram_tensor(name, list(shape), BF16, kind="Internal").ap()
        self.sc = dict(w_in=scr("sc_in", [L, D, IN_COLS]), w_mg=scr("sc_mg", [L, 4, D, D]), w_br=scr("sc_br", [L, 4, W, D]),
                       w_out=scr("sc_out", [L, D, D]), w_glu=scr("sc_glu", [L, W, W]),
                       rsw=scr("sc_rsw", [L, D, 512]),
                       lruw=scr("sc_lruw", [L, 128, 1024]), s5cT=scr("sc_cT", [L, 128, 4096]),
                       wlr=scr("sc_wlr", [L, 16, 256]), idn=scr("sc_idn", [128, 128]))
        self.scres = {k: [Res(f"sc_{k}{l}") for l in range(L)] for k in self.sc}
        self.bbres = [Res(f"bbscr{l}") for l in range(L)]
        self.rotres = [[Res(f"rotscr{l}_{g}") for g in range(16)] for l in range(L)]
        self.dbg_out = {}
        for name, shape in self.debug:
            self.dbg_out[name] = nc.dram_tensor("dbg_" + name, list(shape), F32, kind="ExternalOutput").ap()
        self.dr = dr

        with ExitStack() as stack:
            P = Prog(nc, stack)
            self.P = P
            self.alloc()
            if self.stage >= 1:
                self.convert_weights()
            self.plan_weights()
            if self.stage >= 2:
                self.prologue()
            for sq in range(self.n_seq):
                self.reset_state()
                for ti in range(self.tps):
                    self.tile_step(sq, ti)
            P.finish()
            P.emit()
        return nc

    def alloc(self):
        P = self.P
        L = self.L
        sb = P.sb
        self.h = sb("h", [128, KC, T], F32, KC)
        self.u = sb("u", [128, KC, T], BF16, KC)
        self.mg = sb("mg", [128, KC, T], F32, KC)
        self.NS = 4
        self.SLOT = 8 * 528
        self.ring = [sb(f"ring{i}", [128, self.SLOT], BF16) for i in range(self.NS)]
        self.cst = sb("cst", [128, 1216], F32)
        self.idn = sb("idn", [128, 128], BF16)
        self.ones = sb("ones", [128, 128], BF16)
        self.tpos = sb("tpos", [128, T], F32)
        self.pp = sb("pp", [128, L, NPP], F32)
        self.fg = sb("fg", [128, 8], F32)
        self.lruw = sb("lruw", [128, 2, 4, 128], BF16)
        self.cTw = sb("cTw", [128, 2, 16, 128], BF16)
        self.wlr = sb("wlr", [16, L, 256], BF16)
        self.nblr = sb("nblr", [128, L, 2], F32)
        self.lsc = sb("lsc", [128, L, 4], F32)
        self.rho = sb("rho", [128, L, 16], F32)
        self.g64 = sb("g64", [128, 4], F32)
        self.rt = sb("rt", [128, 2, 4, T], F32, 2)
        self.f = [sb(f"f{i}", [128, T], F32) for i in range(10)]
        self.b = [sb(f"b{i}", [128, T], BF16) for i in range(8)]
        self.sg = sb("sg", [128, NFT, T], F32, NFT)
        self.obr_all = sb("obr", [128, 4 * NFT, T], BF16, 4 * NFT)
        self.obr = None
        self.mgt = [sb(f"mgt{i}", [128, T], F32) for i in range(3)]
        self.itmp = Tile(self.mgt[2].t[:].bitcast(I32), "itmp", 0)
        self.itmp.res = self.mgt[2].res
        self.epsb = sb("epsb", [128, 1], F32)
        self.oneb = sb("oneb", [128, 1], F32)
        self.l8b = sb("l8b", [128, 1], F32)
        MXB = 14848
        self.mx = sb("mx", [128, MXB], BF16)
        raw = self.mx.t

        def view(off_bf16, shape, dt, name, nparts=0):
            n = int(np.prod(shape[1:]))
            nb16 = n * (2 if dt == F32 else 1)
            ap = raw[:, off_bf16:off_bf16 + nb16]
            if dt == F32:
                ap = ap.bitcast(F32)
            if len(shape) == 3:
                ap = ap.rearrange("p (a b) -> p a b", a=shape[1])
            elif len(shape) == 4:
                ap = ap.rearrange("p (a b c) -> p a b c", a=shape[1], b=shape[2])
            return Tile(ap, name, nparts), off_bf16 + nb16

        o = 0
        self.vtok, o = view(o, [128, NTB, 512], BF16, "vtok", NTB)
        self.kttok, o = view(o, [128, NTB, 256], BF16, "kttok", NTB)
        self.sT, o = view(o, [128, NTB, 4, 128], BF16, "sT", NTB)
        self.S32, o = view(o, [128, 2, NCH + 1, 128], F32, "S32", 2)
        self.Sbf, o = view(o, [128, 2, NCH, 128], BF16, "Sbf", 2)
        assert o <= MXB, o
        self.la_res = self.vtok.res + self.kttok.res + self.sT.res + self.S32.res + self.Sbf.res
        o = 0
        self.lxh, o = view(o, [128, NFT, T + 4], F32, "lxh", NFT)
        self.lru_res = self.lxh.res
        o = 0
        self.bbw, o = view(o, [128, 2, 8, 128], BF16, "bbw")
        self.rot0, o = view(o, [128, 2, T], F32, "rot0")
        self.rot1, o = view(o, [128, 2, T], F32, "rot1")
        self.rot = [self.rot0, self.rot1]
        self.Hs, o = view(o, [128, 2, 4, T], BF16, "Hs", 2)
        self.y5, o = view(o, [128, NFT, T], F32, "y5", NFT)
        self.dD, o = view(o, [128, NFT, 128], BF16, "dD")
        assert o <= MXB, o
        self.s5_res = self.bbw.res + self.rot0.res + self.rot1.res + self.Hs.res + self.y5.res + self.dD.res
        self.mx_cur = None
        self.st_gla = sb("st_gla", [128, L, 2, 128], F32, L)
        self.st_ret = sb("st_ret", [128, L, 2, 128], F32, L)
        self.st_lru = sb("st_lru", [128, L, 4], F32, L)
        self.st_lxh = sb("st_lxh", [128, L, 4, 4], F32, L)
        self.st_s5 = sb("st_s5", [128, L, 2, 16], F32, L)
        self.bank = [P.ps(f"bank{i}", [128, T], F32) for i in range(8)]
        self.bk = 0

    def mx_switch(self, which):
        groups = {"la": self.la_res, "lru": self.lru_res, "s5": self.s5_res}
        if self.mx_cur is not None:
            self.P.handoff(groups[self.mx_cur], groups[which])
        self.mx_cur = which

    def nb(self, pool=None):
        if pool is not None:
            ids = pool[0]
            b = self.bank[ids[pool[1] % len(ids)]]
            pool[1] += 1
            return b
        b = self.bank[self.bk]
        self.bk = (self.bk + 1) % 8
        return b

    def convert_weights(self):
        P = self.P
        dr = self.dr

        def flat(ap, pat, n=2048):
            return ap.rearrange(pat).rearrange("(a b) -> a b", b=n)
        for l in range(self.L):
            for k, pat, n in (("w_in", "a b -> (a b)", 2048), ("w_mg", "g a b -> (g a b)", 2048), ("w_br", "g a b -> (g a b)", 2048),
                              ("w_out", "a b -> (a b)", 2048), ("w_glu", "a b -> (a b)", 2048)):
                P.dma("pool", V(flat(self.sc[k][l], pat, n), (self.scres[k][l],)), V(flat(dr[k][l], pat, n), ()))
        if 1 in self.mixers:
            for l in range(self.L):
                src = dr["w_in"][l][:, C_RQ:C_RQ + 512].rearrange("r (g two i) -> r g two i", two=2, i=32)
                dst = self.sc["rsw"][l].rearrange("r (g two i) -> r g two i", two=2, i=32)
                for two in range(2):
                    P.dma("pool", V(dst[:, :, two, :], (self.scres["rsw"][l],)), V(src[:, :, 1 - two, :], ()))
        P.dma("pool", V(self.sc["lruw"].rearrange("l p n -> (l p) n"), tuple(self.scres["lruw"])),
              V(dr["lruw"].rearrange("l p a f n -> (l p) (a f n)"), ()))
        P.dma("pool", V(self.sc["s5cT"].rearrange("l p n -> (l p) n"), tuple(self.scres["s5cT"])),
              V(dr["s5cT"].rearrange("l p a g n -> (l p) (a g n)"), ()))
        P.dma("pool", V(self.sc["wlr"].rearrange("l r n -> (l r) n"), tuple(self.scres["wlr"])),
              V(dr["wlr"].rearrange("l r n -> (l r) n"), ()))
        P.dma("pool", V(self.sc["idn"], tuple(self.scres["idn"])), V(dr["idn"], ()))

    def plan_weights(self):
        dr = self.sc
        rs = self.scres
        self.wplan = []
        for sq in range(self.n_seq):
            for ti in range(self.tps):
                for l in range(self.L):
                    def win(c0, n):
                        return ("in", V(dr["w_in"][l].rearrange("(k p) n -> p k n", p=128)[:, :, c0:c0 + n], (rs["w_in"][l],)), KC, n)

                    def mgs(b):
                        v = dr["w_mg"][l, b].rearrange("(k p) n -> p k n", p=128)
                        br = dr["w_br"][l, b].rearrange("(k p) n -> p k n", p=128)
                        rm, rb_ = (rs["w_mg"][l],), (rs["w_br"][l],)
                        return [("mg", V(v[:, :, 0:512], rm), KC, 512), ("br", V(br[:, :, 0:512], rb_), 4, 512),
                                ("mg", V(v[:, :, 512:1024], rm), KC, 512), ("br", V(br[:, :, 512:1024], rb_), 4, 512)]
                    g = []
                    if 0 in self.mixers:
                        g += [win(C_GLR, 528), win(C_GQ, 512), win(C_GV, 512)]
                    if 1 in self.mixers:
                        g += [win(C_RQ, 512), ("in", V(dr["rsw"][l].rearrange("(k p) n -> p k n", p=128), (rs["rsw"][l],)), KC, 512),
                              win(C_RV, 512), win(C_RG, 512)]
                    if 2 in self.mixers:
                        g += [win(C_LX, 512), win(C_LG, 512)]
                    if 3 in self.mixers:
                        g += [win(C_SU, 512), win(C_SG, 512),
                              ("glu", V(dr["w_glu"][l].rearrange("(k p) n -> p k n", p=128), (rs["w_glu"][l],)), 4, 512)]
                    for b_ in range(4):
                        if b_ in self.mixers:
                            g += mgs(b_)
                    wo = dr["w_out"][l].rearrange("(k p) n -> p k n", p=128)
                    ro = (rs["w_out"][l],)
                    g += [("out", V(wo[:, :, 0:512], ro), KC, 512), ("out", V(wo[:, :, 512:1024], ro), KC, 512)]
                    self.wplan += g
        self.wissued = 0
        self.wused = 0
        self.wfree = 0

    def _wissue(self):
        P = self.P
        lim = min(len(self.wplan), self.wfree + self.NS)
        while self.wissued < lim:
            n = self.wissued
            k, src, nk, ncol = self.wplan[n]
            slot = self.ring[n % self.NS]
            dst = slot.t[:, 0:nk * ncol].rearrange("p (k n) -> p k n", k=nk)
            P.dma("sp", V(dst, slot.res), src)
            self.wissued += 1

    def wnext(self, kind):
        assert self.wused - self.wfree < self.NS
        self._wissue()
        n = self.wused
        k, src, nk, ncol = self.wplan[n]
        assert k == kind, (k, kind)
        assert n < self.wissued
        slot = self.ring[n % self.NS]
        self.wused += 1
        return Tile(slot.t[:, 0:nk * ncol].rearrange("p (k n) -> p k n", k=nk), slot.name, 0), slot.res

    def wdone(self, n=1):
        self.wfree += n
        assert self.wfree <= self.wused
        self._wissue()

    def dbg(self, name, v, idx=None):
        if name in self.dbg_out:
            o = self.dbg_out[name]
            if idx is not None:
                o = o[idx]
            self.P.dma("sp" if v.ap.dtype == F32 else "pool", V(o, ()), v, is_output=True)

    def sin_of(self, x, tmp, out_sin):
        P = self.P
        it = self.itmp.all()
        P.ts("dve", it, x, 1.0 / TWO_PI, None, ALU.mult)
        P.copy("dve", tmp, it)
        P.stt(x, tmp, -CW1, x, ALU.mult, ALU.add)
        P.stt(x, tmp, -CW2, x, ALU.mult, ALU.add)
        P.ts("dve", x, x, math.pi, -math.pi, ALU.min, ALU.max)
        P.act(out_sin, x, AF.Sin)

    def proj_fm(self, ps, wt, wres, c0, ncol, rhs, nk=KC):
        for k in range(nk):
            self.P.mm(ps, V(wt.t[:, k, c0:c0 + ncol], wres), rhs[:, k, :], start=(k == 0), stop=(k == nk - 1))

    def prologue(self):
        P = self.P
        L = self.L
        dr = self.dr
        P.dma("sp", self.cst.all(), V(dr["cst"], ()))
        P.dma("sp", self.tpos.all(), V(dr["tpos"], ()))
        P.dma("sp", self.fg.all(), V(dr["fg"], ()))
        P.dma("sp", self.pp.all(), V(dr["pp"].rearrange("l p n -> p l n"), ()))
        P.dma("sp", self.idn.all(), V(self.sc["idn"], tuple(self.scres["idn"])))
        P.dma("sp", self.wlr.all(), V(self.sc["wlr"].rearrange("l r n -> r l n"), tuple(self.scres["wlr"])))
        P.memset("dve", self.ones.all(), 1.0)
        P.memset("dve", self.epsb.all(), EPS)
        P.memset("dve", self.oneb.all(), 1.0)
        P.memset("dve", self.l8b.all(), math.log(0.125))
        f = self.f
        for l in range(L):
            P.ts("dve", self.nblr[:, l, :], self.pp[:, l, 8:10], -1.0, None, ALU.mult)
            t4 = f[0][:, 0:4]
            P.act(t4, self.pp[:, l, 50:54], AF.Exp, scale=-1.0)
            P.act(t4, t4, AF.Ln, bias=self.oneb[:, 0:1])
            P.ts("dve", self.lsc[:, l, :], t4, -8.0, None, ALU.mult)
        for tile in range(2):
            lg = self.cst[:, 1153 + tile:1154 + tile]
            P.act(self.g64[:, tile:tile + 1], lg, AF.Exp, scale=64.0)
            P.ts("dve", self.g64[:, 2 + tile:3 + tile], lg, -1.0, None, ALU.mult)
            pass
        if 3 in self.mixers:
            self.mx_cur = "s5"
            self.s5_prep(0)
        self.prepped = 1

    def s5_prep(self, l):
        P = self.P
        dr = self.dr
        f = self.f
        step = f[0][:, 0:16]
        theta = f[0][:, 16:32]
        P.act(step, self.pp[:, l, 94:110], AF.Exp)
        P.tt("dve", f[0][:, 32:48], step, self.pp[:, l, 62:78], ALU.mult)
        P.act(self.rho[:, l, :], f[0][:, 32:48], AF.Exp)
        P.tt("dve", theta, step, self.pp[:, l, 78:94], ALU.mult)
        for gp in range(16):
            ang, ang2, tmp = f[1], f[2], f[3]
            rb = self.rot[gp % 2]
            P.ts("dve", ang.all(), self.tpos.all(), 1.0, theta[:, gp:gp + 1], ALU.add, ALU.mult)
            P.ts("dve", ang2.all(), ang.all(), math.pi / 2.0, None, ALU.add)
            self.sin_of(ang.all(), tmp.all(), rb[:, 1, :])
            self.sin_of(ang2.all(), tmp.all(), rb[:, 0, :])
            P.dma("sp", V(self.rotscr[l, gp], (self.rotres[l][gp],)), rb.all())
        bbv = self.bbw
        for e in range(2):
            def half(src):
                return V(src.rearrange("p (a e n) -> p a e n", a=4, e=2)[:, :, e, :], ())

            def t3(v):
                return V(v.ap.rearrange("p (a n) -> p a n", a=4), v.res)
            are, aim, stp, bre, bim = (f[k].all() for k in range(4, 9))
            P.dma("sp", t3(are), half(dr["s5aT"][l, 0]))
            P.dma("sp", t3(aim), half(dr["s5aT"][l, 1]))
            P.dma("sp", t3(stp), half(dr["s5aT"][l, 2]))
            P.dma("sp", t3(bre), half(dr["s5bT"][l, 0]))
            P.dma("sp", t3(bim), half(dr["s5bT"][l, 1]))
            t0, t1, t2, t3_, t4 = f[0].all(), f[1].all(), f[2].all(), f[3].all(), f[9].all()
            P.act(stp, stp, AF.Exp)
            P.tt("dve", t0, stp, are, ALU.mult)
            P.act(t0, t0, AF.Exp)
            P.tt("dve", t1, stp, aim, ALU.mult)
            P.ts("dve", t2, t1, math.pi / 2.0, None, ALU.add)
            self.sin_of(t1, t3_, t1)
            self.sin_of(t2, t3_, t2)
            P.tt("dve", t1, t1, t0, ALU.mult)
            P.tt("dve", t2, t2, t0, ALU.mult)
            P.ts("dve", t2, t2, -1.0, None, ALU.add)
            P.tt("dve", t0, are, are, ALU.mult)
            P.tt("dve", t3_, aim, aim, ALU.mult)
            P.tt("dve", t0, t0, t3_, ALU.add)
            P.recip(t0, t0)
            P.tt("dve", t3_, t2, are, ALU.mult)
            P.tt("dve", t4, t1, aim, ALU.mult)
            P.tt("dve", t3_, t3_, t4, ALU.add)
            P.tt("dve", t3_, t3_, t0, ALU.mult)
            P.tt("dve", t4, t1, are, ALU.mult)
            P.tt("dve", t2, t2, aim, ALU.mult)
            P.tt("dve", t4, t4, t2, ALU.subtract)
            P.tt("dve", t4, t4, t0, ALU.mult)
            P.tt("dve", t0, t3_, bre, ALU.mult)
            P.tt("dve", t1, t4, bim, ALU.mult)
            P.tt("dve", t0, t0, t1, ALU.subtract)
            P.tt("dve", t1, t3_, bim, ALU.mult)
            P.tt("dve", t2, t4, bre, ALU.mult)
            P.tt("dve", t1, t1, t2, ALU.add)
            bb4 = bbv.t[:].rearrange("p r (a e) n -> p r a e n", e=2)
            P.copy("act", V(bb4[:, 0, :, e, :], bbv.res), V(t0.ap.rearrange("p (a n) -> p a n", a=4), t0.res))
            P.copy("act", V(bb4[:, 1, :, e, :], bbv.res), V(t1.ap.rearrange("p (a n) -> p a n", a=4), t1.res))
        P.dma("sp", V(self.bbscr[l], (self.bbres[l],)), V(bbv.t[:].rearrange("p r a n -> p r (a n)"), bbv.res))
        for ft in range(NFT):
            P.ts("dve", self.dD[:, ft, :], self.idn.all(), self.pp[:, l, 54 + ft:55 + ft], None, ALU.mult)
        P.dma("sp", V(self.ddscr[l], (self.ddres[l],)), V(self.dD.t[:].rearrange("p a n -> p (a n)"), self.dD.res))

    def reset_state(self):
        P = self.P
        for t in (self.st_gla, self.st_ret, self.st_lru, self.st_lxh, self.st_s5):
            P.memset("pool", t.all(), 0.0)

    def tile_step(self, sq, ti):
        P = self.P
        t0 = ti * T
        xsrc = self.dr["xT"][sq].rearrange("(k p) s -> p k s", p=128)[:, :, t0:t0 + T]
        P.dma("sp", self.h.all(), V(xsrc, ()))
        if 1 in self.mixers:
            self.ret_tables(t0)
        for l in range(self.L):
            if self.stage >= 3:
                if 3 in self.mixers and self.prepped <= l + 1 < self.L:
                    self.P.cur = "prep"
                    self.mx_switch("s5")
                    self.s5_prep(l + 1)
                    self.prepped = l + 2
                self.layer(l)
        ydst = self.yT[sq].rearrange("(k p) s -> p k s", p=128)[:, :, t0:t0 + T]
        if self.final_norm:
            rstd = self.rms_rstd(self.h, D)
            for k in range(KC):
                P.stt(self.mg[:, k, :], self.h[:, k, :], self.fg[:, k:k + 1], rstd, ALU.mult, ALU.mult)
            P.dma("sp", V(ydst, ()), self.mg.all(), is_output=True)
        else:
            P.dma("sp", V(ydst, ()), self.h.all(), is_output=True)

    def ret_tables(self, t0):
        P = self.P
        f = self.f
        ang, ang2, tmp, sn, cs = f[0].all(), f[1].all(), f[2].all(), f[3].all(), f[4].all()
        P.ts("dve", ang, self.tpos.all(), float(t0), self.cst[:, 1152:1153], ALU.add, ALU.mult)
        P.ts("dve", ang2, ang, math.pi / 2.0, None, ALU.add)
        self.sin_of(ang, tmp, sn)
        self.sin_of(ang2, tmp, cs)
        P.ts("dve", sn, sn, self.cst[:, 1155:1156], None, ALU.mult)
        for tile in range(2):
            dqq, dqk = f[5].all(), f[6].all()
            P.act(dqq, self.cst[:, 640:1152], AF.Exp, scale=self.cst[:, 1153 + tile:1154 + tile])
            P.act(dqk, self.cst[:, 640:1152], AF.Exp, scale=self.g64[:, 2 + tile:3 + tile], bias=self.l8b[:, 0:1])
            P.tt("dve", self.rt[:, tile, 0, :], cs, dqq, ALU.mult)
            P.tt("dve", self.rt[:, tile, 1, :], sn, dqq, ALU.mult)
            P.tt("dve", self.rt[:, tile, 2, :], cs, dqk, ALU.mult)
            P.tt("dve", self.rt[:, tile, 3, :], sn, dqk, ALU.mult)

    def rms_rstd(self, src, n):
        P = self.P
        ps = self.nb()
        for k in range(KC):
            sq = self.b[k % 2]
            P.act(sq.all(), src[:, k, :], AF.Square)
            P.mm(ps.all(), self.ones.all(), sq.all(), start=(k == 0), stop=(k == KC - 1))
        r = self.f[9].all()
        P.act(r, ps.all(), AF.Ln, bias=self.epsb[:, 0:1], scale=1.0 / n)
        P.act(r, r, AF.Exp, scale=-0.5)
        return r

    def layer(self, l):
        P = self.P
        self.bk = 0
        P.cur = "norm"
        rstd = self.rms_rstd(self.h, D)
        for k in range(KC):
            P.stt(self.u[:, k, :], self.h[:, k, :], self.pp[:, l, k:k + 1], rstd, ALU.mult, ALU.mult)
        self.first_branch = True
        self.dbg("u", self.u.all())
        if self.stage < 4:
            return
        for b, name, fn in ((0, "gla", self.gla), (1, "ret", self.ret), (2, "lru", self.lru), (3, "s5", self.s5)):
            if b in self.mixers:
                P.cur = name
                self.obr = Tile(self.obr_all.t[:, b * NFT:(b + 1) * NFT, :], f"obr{b}", 0)
                self.obr.res = self.obr_all.res[b * NFT:(b + 1) * NFT]
                self.obr.parts = True
                fn(l)
                if b == 0:
                    self.dbg("obr", self.obr.all())
        if self.stage < 7:
            return
        self.mpool = [[5, 6, 7], 0]
        for b in range(4):
            if b in self.mixers:
                P.cur = f"merge{b}"
                self.obr = Tile(self.obr_all.t[:, b * NFT:(b + 1) * NFT, :], f"obr{b}", 0)
                self.obr.res = self.obr_all.res[b * NFT:(b + 1) * NFT]
                self.obr.parts = True
                self.merge(l)
        if self.stage < 8:
            return
        P.cur = "outproj"
        self.outproj(l)

    def merge(self, l):
        P = self.P
        for dm in range(KC):
            if dm % 4 == 0:
                if dm:
                    self.wdone(2)
                m, r = self.wnext("mg")
                wb, rb = self.wnext("br")
            gps = self.nb(self.mpool)
            bps = self.nb(self.mpool)
            c0 = (dm % 4) * 128
            self.proj_fm(gps.all(), m, r, c0, 128, self.u)
            self.proj_fm(bps.all(), wb, rb, c0, 128, self.obr, nk=4)
            g = self.mgt[dm % 2].all()
            P.act(g, gps.all(), AF.Sigmoid)
            if self.first_branch:
                P.tt("dve", self.mg[:, dm, :], g, bps.all(), ALU.mult)
            else:
                tmp = self.mgt[2].all()
                P.tt("dve", tmp, g, bps.all(), ALU.mult)
                P.tt("pool", self.mg[:, dm, :], self.mg[:, dm, :], tmp, ALU.add)
        self.wdone(2)
        self.first_branch = False

    def outproj(self, l):
        P = self.P
        mb = self.u
        for k in range(KC):
            P.copy("act", mb[:, k, :], self.mg[:, k, :])
        for dm in range(KC):
            if dm % 4 == 0:
                if dm:
                    self.wdone(1)
                w, r = self.wnext("out")
            ps = self.nb()
            self.proj_fm(ps.all(), w, r, (dm % 4) * 128, 128, mb)
            P.tt("dve", self.h[:, dm, :], self.h[:, dm, :], ps.all(), ALU.add)
        self.wdone(1)

    def la_core(self, segs, qd, kd, kt, decay, st, norm):
        P = self.P
        tb_bank = self.nb()
        tbv = tb_bank.t[:].bitcast(BF16)
        for tb in range(NTB):
            for tile in range(2):
                c0 = (tb * 2 + tile) * 128
                P.tr(V(tbv[:, c0:c0 + 128], tb_bank.res), kt[tile][:, tb * 128:(tb + 1) * 128], self.idn.all())
        P.copy("act", V(self.kttok.t[:].rearrange("p a n -> p (a n)"), self.kttok.res), V(tbv, tb_bank.res))
        if self.stage < 5.1:
            return
        kvb = [[self.nb(), self.nb()] for _ in range(2)]
        for tile in range(2):
            for h in range(4):
                for (tl, r0, K) in segs[h]:
                    if tl != tile:
                        continue
                    for c in range(NCH):
                        tb, half = c // 2, c % 2
                        bank = kvb[tile][half]
                        cc = tb * 128
                        P.mm(V(bank.t[r0:r0 + K, cc:cc + 128], bank.res),
                             V(self.kttok.t[half * 64:(half + 1) * 64, tb, tile * 128 + r0:tile * 128 + r0 + K], (self.kttok.res[tb],)),
                             V(self.vtok.t[half * 64:(half + 1) * 64, tb, h * 128:(h + 1) * 128], (self.vtok.res[tb],)),
                             start=True, stop=True)
        if self.stage < 5.2:
            return
        for tile in range(2):
            P.copy("pool", self.S32[:, tile, 0, :], st[:, tile, :])
            for c in range(NCH):
                bank = kvb[tile][c % 2]
                cc = (c // 2) * 128
                P.stt(self.S32[:, tile, c + 1, :], self.S32[:, tile, c, :], decay(tile, c),
                      V(bank.t[:, cc:cc + 128], bank.res), ALU.mult, ALU.add)
            P.copy("pool", st[:, tile, :], self.S32[:, tile, NCH, :])
            P.copy("act", self.Sbf[:, tile, :, :], V(self.S32.t[:, tile, 0:NCH, :], (self.S32.res[tile],)))
        if self.stage < 5.3:
            return
        for h in range(4):
            sps = self.nb()
            n = len(segs[h])
            for tb in range(NTB):
                for si, (tl, r0, K) in enumerate(segs[h]):
                    P.mm(V(sps.t[:, tb * 128:(tb + 1) * 128], sps.res),
                         V(kd[tl].ap[r0:r0 + K, tb * 128:(tb + 1) * 128], kd[tl].res),
                         V(qd[tl].ap[r0:r0 + K, tb * 128:(tb + 1) * 128], qd[tl].res),
                         start=(si == 0), stop=(si == n - 1))
            for tb in range(NTB):
                P.tt("dve", V(self.sT.t[:, tb, h, :], (self.sT.res[tb],)), V(sps.t[:, tb * 128:(tb + 1) * 128], sps.res),
                     self.cst[:, 0:128], ALU.mult)
        if self.stage < 5.4:
            return
        for h in range(4):
            ops = self.nb()
            for tb in range(NTB):
                P.mm(V(ops.t[:, tb * 128:(tb + 1) * 128], ops.res),
                     V(self.vtok.t[:, tb, h * 128:(h + 1) * 128], (self.vtok.res[tb],)),
                     V(self.sT.t[:, tb, h, :], (self.sT.res[tb],)), start=True, stop=False)
                for half in range(2):
                    c = tb * 2 + half
                    n = len(segs[h])
                    for si, (tl, r0, K) in enumerate(segs[h]):
                        P.mm(V(ops.t[:, c * 64:(c + 1) * 64], ops.res),
                             V(self.Sbf.t[r0:r0 + K, tl, c, :], (self.Sbf.res[tl],)),
                             V(qd[tl].ap[r0:r0 + K, c * 64:(c + 1) * 64], qd[tl].res),
                             start=False, stop=(half == 1 and si == n - 1))
            if self.stage >= 5.5:
                norm(h, ops)

    def gates(self, wg, rg, g0):
        P = self.P
        for ft in range(NFT):
            ps = self.nb()
            self.proj_fm(ps.all(), wg, rg, g0 + ft * 128, 128, self.u)
            P.act(self.sg[:, ft, :], ps.all(), AF.Silu)

    def vproj(self, wv, rv):
        P = self.P
        for tb in range(NTB):
            ps = self.nb()
            for k in range(KC):
                P.mm(ps.all(), self.u[:, k, tb * 128:(tb + 1) * 128], V(wv.t[:, k, 0:512], rv),
                     start=(k == 0), stop=(k == KC - 1))
            P.copy("act", self.vtok[:, tb, :], ps.all())

    def gla(self, l):
        P = self.P
        f, b = self.f, self.b
        self.mx_switch("la")
        w1, r1 = self.wnext("in")
        lrp = self.nb()
        self.proj_fm(V(lrp.t[0:16, :], lrp.res), w1, r1, 0, 16, self.u)
        self.gates(w1, r1, 16)
        self.wdone(1)
        w2, r2 = self.wnext("in")
        lrb = V(b[0].t[0:16, :], b[0].res)
        P.copy("act", lrb, V(lrp.t[0:16, :], lrp.res))
        eq = [f[0].all(), f[1].all()]
        ek = [f[2].all(), f[3].all()]
        for tile in range(2):
            zp = self.nb()
            P.mm(zp.all(), V(self.wlr.t[0:16, l, tile * 128:(tile + 1) * 128], self.wlr.res), lrb, start=True, stop=True)
            e1 = f[4].all()
            P.act(e1, zp.all(), AF.Exp, bias=self.nblr[:, l, tile:tile + 1], scale=-1.0)
            P.act(e1, e1, AF.Ln, bias=self.oneb[:, 0:1])
            cum = f[5].all()
            P.scan(cum, self.cst[:, 128:640], e1, 0.0)
            P.act(eq[tile], cum, AF.Exp, scale=-1.0 / 16.0)
            P.act(ek[tile], cum, AF.Exp, scale=1.0 / 16.0)
        qd = [b[1].all(), b[2].all()]
        kd = [b[3].all(), b[4].all()]
        kt = [b[5].all(), b[6].all()]
        for tile in range(2):
            qp = self.nb()
            self.proj_fm(qp.all(), w2, r2, tile * 128, 128, self.u)
            P.stt(qd[tile], qp.all(), 0.125, eq[tile], ALU.mult, ALU.mult)
            kp = self.nb()
            self.proj_fm(kp.all(), w2, r2, 256 + tile * 128, 128, self.u)
            kd32 = f[6 + tile].all()
            P.tt("dve", kd32, kp.all(), ek[tile], ALU.mult)
            P.copy("act", kd[tile], kd32)
            eql = V(eq[tile].ap.rearrange("p (c t) -> p c t", t=64)[:, :, 63:64].to_broadcast([128, NCH, 64]), eq[tile].res)
            P.tt("dve", V(kt[tile].ap.rearrange("p (c t) -> p c t", t=64), kt[tile].res),
                 V(kd32.ap.rearrange("p (c t) -> p c t", t=64), kd32.res), eql, ALU.mult)
        self.wdone(1)
        w3, r3 = self.wnext("in")
        self.vproj(w3, r3)
        self.wdone(1)
        self.dbg("sg", self.sg.all())
        self.dbg("qd0", qd[0])
        self.dbg("kd0", kd[0])
        self.dbg("kt0", kt[0])
        self.dbg("eq0", eq[0])
        self.dbg("vtok", self.vtok.all())

        if self.stage < 5:
            return

        def decay(tile, c):
            return eq[tile][:, c * 64 + 63:c * 64 + 64]

        def norm(h, ops):
            osq = b[7].all()
            P.act(osq, ops.all(), AF.Square)
            o32 = f[8].all()
            P.copy("act", o32, ops.all())
            sp = self.nb()
            P.mm(sp.all(), self.ones.all(), osq, start=True, stop=True)
            rs = f[9].all()
            P.act(rs, sp.all(), AF.Ln, bias=self.epsb[:, 0:1], scale=1.0 / 128.0)
            P.act(rs, rs, AF.Exp, scale=-0.5)
            P.stt(o32, o32, self.pp[:, l, 10 + h:11 + h], rs, ALU.mult, ALU.mult)
            P.tt("dve", self.obr[:, h, :], o32, self.sg[:, h, :], ALU.mult)

        self.la_core(GLA_SEGS, qd, kd, kt, decay, V(self.st_gla.t[:, l], (self.st_gla.res[l],)), norm)

    def ret(self, l):
        P = self.P
        f, b = self.f, self.b
        self.mx_switch("la")
        w1, r1 = self.wnext("in")
        ws, rs_ = self.wnext("in")
        qd = [b[1].all(), b[2].all()]
        kd = [b[3].all(), b[4].all()]
        kt = [b[5].all(), b[6].all()]
        kd32 = [f[6].all(), f[7].all()]
        for a in range(2):
            for tile in range(2):
                pn = self.nb()
                self.proj_fm(pn.all(), w1, r1, a * 256 + tile * 128, 128, self.u)
                px = self.nb()
                self.proj_fm(px.all(), ws, rs_, a * 256 + tile * 128, 128, self.u)
                cs, sn = self.rt[:, tile, 2 * a, :], self.rt[:, tile, 2 * a + 1, :]
                m1, m2 = f[0 + 2 * tile].all(), f[1 + 2 * tile].all()
                P.tt("dve", m1, pn.all(), cs, ALU.mult)
                P.tt("dve", m2, px.all(), sn, ALU.mult)
                if a == 0:
                    P.tt("pool", qd[tile], m1, m2, ALU.add)
                else:
                    P.tt("pool", kd32[tile], m1, m2, ALU.add)
                    P.copy("act", kd[tile], kd32[tile])
                    P.act(kt[tile], kd32[tile], AF.Copy, scale=self.g64[:, tile:tile + 1])
        self.wdone(1)
        self.wdone(1)
        w2, r2 = self.wnext("in")
        self.vproj(w2, r2)
        self.wdone(1)
        w3, r3 = self.wnext("in")
        self.gates(w3, r3, 0)
        self.wdone(1)

        def decay(tile, c):
            return self.g64[:, tile:tile + 1]

        def norm(h, ops):
            ob, osq = b[7].all(), b[0].all()
            P.copy("act", ob, ops.all())
            P.act(osq, ops.all(), AF.Square)
            o32 = f[8].all()
            P.copy("act", o32, ops.all())
            s1 = self.nb()
            s2 = self.nb()
            P.mm(s1.all(), self.ones.all(), ob, start=True, stop=True)
            P.mm(s2.all(), self.ones.all(), osq, start=True, stop=True)
            mean, var = f[4].all(), f[5].all()
            P.act(mean, s1.all(), AF.Copy, scale=1.0 / 128.0)
            P.tt("dve", var, mean, mean, ALU.mult)
            P.stt(var, s2.all(), 1.0 / 128.0, var, ALU.mult, ALU.subtract)
            P.act(var, var, AF.Ln, bias=self.epsb[:, 0:1])
            P.act(var, var, AF.Exp, scale=-0.5)
            P.tt("dve", o32, o32, mean, ALU.subtract)
            P.stt(o32, o32, self.pp[:, l, 14 + h:15 + h], var, ALU.mult, ALU.mult)
            P.stt(self.obr[:, h, :], o32, self.pp[:, l, 18 + h:19 + h], self.sg[:, h, :], ALU.add, ALU.mult)

        self.la_core(GLA_SEGS, qd, kd, kt, decay, V(self.st_ret.t[:, l], (self.st_ret.res[l],)), norm)

    def lru(self, l):
        P = self.P
        f, b = self.f, self.b
        self.mx_switch("lru")
        w1, r1 = self.wnext("in")
        P.dma("sp", V(self.lruw.t[:].rearrange("p a f n -> p (a f n)"), self.lruw.res), V(self.sc["lruw"][l], (self.scres["lruw"][l],)))
        stl = (self.st_lru.res[l],)
        sth = (self.st_lxh.res[l],)
        for ft in range(NFT):
            ps = self.nb()
            self.proj_fm(ps.all(), w1, r1, ft * 128, 128, self.u)
            P.copy("pool", V(self.lxh.t[:, ft, 0:3], (self.lxh.res[ft],)), V(self.st_lxh.t[:, l, ft, 0:3], sth))
            P.copy("act", V(self.lxh.t[:, ft, 3:3 + T], (self.lxh.res[ft],)), ps.all())
            P.copy("pool", V(self.st_lxh.t[:, l, ft, 0:3], sth), V(self.lxh.t[:, ft, T:T + 3], (self.lxh.res[ft],)))
        self.wdone(1)
        w2, r2 = self.wnext("in")
        self.gates(w2, r2, 0)
        self.wdone(1)
        for ft in range(NFT):
            xc = f[0 + (ft % 2) * 5].all()
            cw = lambda w: self.pp[:, l, 22 + ft * 4 + w:23 + ft * 4 + w]
            P.ts("dve", xc, V(self.lxh.t[:, ft, 0:T], (self.lxh.res[ft],)), cw(0), self.pp[:, l, 38 + ft:39 + ft], ALU.mult, ALU.add)
            for w in range(1, 4):
                P.stt(xc, V(self.lxh.t[:, ft, w:w + T], (self.lxh.res[ft],)), cw(w), xc, ALU.mult, ALU.add)
            xcb = b[ft % 2].all()
            P.copy("act", xcb, xc)
            rp = self.nb()
            ip = self.nb()
            P.mm(rp.all(), V(self.lruw.t[:, 0, ft, :], self.lruw.res), xcb, start=True, stop=True)
            P.mm(ip.all(), V(self.lruw.t[:, 1, ft, :], self.lruw.res), xcb, start=True, stop=True)
            a, mu, ig = f[1 + (ft % 2) * 5].all(), f[2 + (ft % 2) * 5].all(), f[3 + (ft % 2) * 5].all()
            P.act(a, rp.all(), AF.Sigmoid, bias=self.pp[:, l, 42 + ft:43 + ft])
            P.act(a, a, AF.Exp, scale=self.lsc[:, l, ft:ft + 1])
            P.act(mu, a, AF.Square)
            P.act(mu, mu, AF.Sqrt, bias=self.oneb[:, 0:1], scale=-1.0)
            P.act(ig, ip.all(), AF.Sigmoid, bias=self.pp[:, l, 46 + ft:47 + ft])
            P.tt("dve", ig, ig, xc, ALU.mult)
            P.tt("dve", ig, ig, mu, ALU.mult)
            hs = f[4 + (ft % 2) * 5].all()
            P.scan(hs, a, ig, V(self.st_lru.t[:, l, ft:ft + 1], stl))
            P.copy("pool", V(self.st_lru.t[:, l, ft:ft + 1], stl), hs[:, T - 1:T])
            P.tt("pool", self.obr[:, ft, :], hs, self.sg[:, ft, :], ALU.mult)

    def s5(self, l):
        P = self.P
        f, b = self.f, self.b
        self.mx_switch("s5")
        w1, r1 = self.wnext("in")
        P.dma("sp", V(self.bbw.t[:].rearrange("p r a n -> p r (a n)"), self.bbw.res), V(self.bbscr[l], (self.bbres[l],)))
        P.dma("sp", V(self.cTw.t[:].rearrange("p a g n -> p (a g n)"), self.cTw.res), V(self.sc["s5cT"][l], (self.scres["s5cT"][l],)))
        P.dma("sp", V(self.dD.t[:].rearrange("p a n -> p (a n)"), self.dD.res), V(self.ddscr[l], (self.ddres[l],)))
        P.act(self.cTw[:, 1], self.cTw[:, 1], AF.Copy, scale=-1.0)
        sts = (self.st_s5.res[l],)
        sub = [b[k].all() for k in range(4)]
        for ft in range(NFT):
            ps = self.nb()
            self.proj_fm(ps.all(), w1, r1, ft * 128, 128, self.u)
            P.copy("act", sub[ft], ps.all())
        self.wdone(1)
        w2, r2 = self.wnext("in")
        self.gates(w2, r2, 0)
        self.wdone(1)
        y5b = [b[4 + k].all() for k in range(4)]
        yps = None
        bupool = [[0, 1, 2, 3], 0]
        ypool = [[4], 0]
        for gp in range(16):
            ft, q = gp // 4, gp % 4
            q2, e = q // 2, q % 2
            par = gp % 2
            rb = self.rot[par]
            P.dma("sp", rb.all(), V(self.rotscr[l, gp], (self.rotres[l][gp],)))
            C, Sn = rb[:, 0, :], rb[:, 1, :]
            brp, bip = self.nb(bupool), self.nb(bupool)
            rows = slice(64 * q2, 64 * q2 + 64)
            P.mm(brp.all(), V(self.bbw.t[rows, 0, ft * 2 + e, :], self.bbw.res), V(sub[ft].ap[rows, :], sub[ft].res), start=True, stop=True)
            P.mm(bip.all(), V(self.bbw.t[rows, 1, ft * 2 + e, :], self.bbw.res), V(sub[ft].ap[rows, :], sub[ft].res), start=True, stop=True)
            t1, t2, t3, t4 = (f[4 * par + k].all() for k in range(4))
            P.tt("dve", t1, brp.all(), C, ALU.mult)
            P.tt("dve", t2, bip.all(), Sn, ALU.mult)
            P.tt("pool", t1, t1, t2, ALU.add)
            P.tt("dve", t3, bip.all(), C, ALU.mult)
            P.tt("dve", t4, brp.all(), Sn, ALU.mult)
            P.tt("pool", t3, t3, t4, ALU.subtract)
            rho_bc = V(self.rho.t[:, l, gp:gp + 1].to_broadcast([128, T]), self.rho.res)
            P.scan(t2, rho_bc, t1, V(self.st_s5.t[:, l, 0, gp:gp + 1], sts))
            P.scan(t4, rho_bc, t3, V(self.st_s5.t[:, l, 1, gp:gp + 1], sts))
            tl = slice(T - 1, T)
            P.tt("dve", self.Hs[:, par, 0, :], t2, C, ALU.mult)
            P.stt(self.Hs[:, par, 1, :], t4, -1.0, Sn, ALU.mult, ALU.mult)
            P.tt("dve", self.Hs[:, par, 2, :], t4, C, ALU.mult)
            P.tt("pool", self.Hs[:, par, 3, :], t2, Sn, ALU.mult)
            ca, cb, cc = t1[:, 0:1], t1[:, 1:2], t1[:, 2:3]
            P.act(ca, t4[:, tl], AF.Copy, scale=Sn[:, tl])
            P.act(cb, t2[:, tl], AF.Copy, scale=C[:, tl])
            P.act(V(self.st_s5.t[:, l, 0, gp:gp + 1], sts), ca, AF.Identity, scale=-1.0, bias=cb)
            P.act(cc, t2[:, tl], AF.Copy, scale=Sn[:, tl])
            P.act(V(self.st_s5.t[:, l, 1, gp:gp + 1], sts), t4[:, tl], AF.Identity, scale=C[:, tl], bias=cc)
            if q == 0:
                yps = self.nb(ypool)
            if q == 0:
                P.mm(yps.all(), self.dD[:, ft, :], sub[ft], start=True, stop=False)
            for k, wsel in enumerate((0, 0, 1, 1)):
                P.mm(yps.all(), V(self.cTw.t[:, wsel, gp, :], self.cTw.res), self.Hs[:, par, k, :], start=False, stop=(q == 3 and k == 3))
            if q == 3:
                y1, y2, inner, sgm = f[8].all(), f[9].all(), f[8].all(), f[9].all()
                yf = self.y5[:, ft, :]
                P.act(y2, yps.all(), AF.Square)
                P.ts("dve", y2, y2, 0.044715, 1.0, ALU.mult, ALU.add)
                P.tt("dve", y1, y2, yps.all(), ALU.mult)
                P.act(sgm, y1, AF.Sigmoid, scale=2.0 * GELU_C)
                P.tt("dve", yf, sgm, yps.all(), ALU.mult)
                P.copy("act", y5b[ft], yf)
        w3, r3 = self.wnext("glu")
        for fo in range(NFT):
            ps = self.nb(bupool)
            for k in range(4):
                P.mm(ps.all(), V(w3.t[:, k, fo * 128:(fo + 1) * 128], r3), y5b[k], start=(k == 0), stop=(k == 3))
            sig = f[8 + fo % 2].all()
            P.act(sig, ps.all(), AF.Sigmoid, bias=self.pp[:, l, 58 + fo:59 + fo])
            P.tt("dve", sig, sig, self.y5[:, fo, :], ALU.mult)
            P.tt("pool", self.obr[:, fo, :], sig, self.sg[:, fo, :], ALU.mult)
        self.wdone(1)


_CACHE = {}


def _get_nc(key, **kw):
    if key not in _CACHE:
        _CACHE[key] = Builder(**kw).build()
    return _CACHE[key]


def _weights_maps(inputs, layers):
    pk = _pack_params(inputs, layers)
    c, idn, tpos = _consts()
    f32 = lambda a: np.ascontiguousarray(np.asarray(a, dtype=np.float32))
    m = dict(pk)
    m["w_in"] = f32(np.asarray(inputs["w_in"])[layers])
    m["w_mg"] = f32(np.asarray(inputs["w_merge_gate"])[layers])
    m["w_br"] = f32(np.asarray(inputs["w_branch"])[layers])
    m["w_out"] = f32(np.asarray(inputs["w_out"])[layers])
    m["w_glu"] = f32(np.asarray(inputs["s5_glu_w"])[layers])
    m["fg"] = f32(np.asarray(inputs["final_norm_gain"]).reshape(8, 128).T)
    m["cst"] = c
    m["idn"] = idn
    m["tpos"] = tpos
    return m


def kernel(**inputs):
    x = np.asarray(inputs["x"], dtype=np.float32)
    B, S, _ = x.shape
    n_cores = 8
    n_seq = B // n_cores
    layers = list(range(DEPTH))
    nc = _get_nc(("full", n_seq, S), n_layers=DEPTH, n_seq=n_seq, tiles_per_seq=S // T, final_norm=True)
    wm = _weights_maps(inputs, layers)
    in_maps = []
    for c in range(n_cores):
        m = dict(wm)
        m["xT"] = np.ascontiguousarray(x[c * n_seq:(c + 1) * n_seq].transpose(0, 2, 1))
        in_maps.append(m)
    res = run_bass_kernel_spmd(nc, in_maps, core_ids=list(range(n_cores)))
    out = np.empty((B, S, D), np.float32)
    for c in range(n_cores):
        out[c * n_seq:(c + 1) * n_seq] = res.results[c]["yT"].transpose(0, 2, 1)
    return out
```
